# Optimizing a Trainium2 kernel written in Bass

```python
import jax, jax.numpy as jnp
from jax import lax
import numpy as np

D_MODEL = 1024
BATCH = 8
SEQ = 4096
DEPTH = 1

CHUNK = 64
Q_BLOCK = 128
POOL_WINDOWS = (2, 4, 8, 16)
POOL_GROUPS = len(POOL_WINDOWS)
POOL_WIDTH = D_MODEL
POOL_GROUP_W = POOL_WIDTH // POOL_GROUPS
MLA_HEADS = 8
QK_NOPE = 128
QK_ROPE = 64
V_HEAD = 128
Q_LORA = 3 * D_MODEL // 4
KV_LORA = D_MODEL // 4
ROPE_THETA = 10000.0
N_BRANCHES = 2
IN_WIDTH = POOL_WIDTH + Q_LORA + KV_LORA + QK_ROPE + N_BRANCHES * D_MODEL
N_EXPERTS = 32
TOP_K = 4
D_EXPERT = D_MODEL
SWIGLU_LIMIT = 7.0
SWIGLU_ALPHA = 1.702
EXPERT_BLOCK = 128
N_MOD = 6
NORM_EPS = 1e-6
NEG_INF = -1e30

kernel_name = 'hybrid_pool_mla_moe_adaln_block'


def rms_norm(x, g):
    xf = x.astype(jnp.float32)
    y = xf * lax.rsqrt(jnp.mean(xf * xf, axis=-1, keepdims=True) + NORM_EPS)
    return (y * g.astype(jnp.float32)).astype(x.dtype)


def modulate(h, shift, scale):
    return h * (1 + scale[:, None, :]) + shift[:, None, :]


def pool_mixer(u, w_grp, scale, w_proj):
    B, S, _ = u.shape
    uf = u.astype(jnp.float32).reshape(B, S, POOL_GROUPS, POOL_GROUP_W)
    csum = jnp.concatenate([jnp.zeros_like(uf[:, :1]), jnp.cumsum(uf, axis=1)], axis=1)
    t = np.arange(S)
    pooled = []
    for g, w in enumerate(POOL_WINDOWS):
        lo = np.maximum(t + 1 - w, 0)
        cnt = np.minimum(t + 1, w).astype(np.float32)
        win = csum[:, 1:, g] - csum[:, lo, g]
        pooled.append(win / cnt[None, :, None])
    pooled = jnp.stack(pooled, axis=2)
    mixed = (pooled - uf).astype(u.dtype)
    y = jnp.einsum('bsgc,gcd->bsgd', mixed, w_grp).reshape(B, S, POOL_WIDTH) * scale
    return y @ w_proj


def rotate(x, cos, sin):
    x1, x2 = jnp.split(x, 2, axis=-1)
    return jnp.concatenate([x1 * cos - x2 * sin, x2 * cos + x1 * sin], axis=-1)


def mla_mixer(q_lat, kv_lat, k_pe_raw, positions, g_q_a, w_q_b, g_kv_a, w_kv_b, w_o):
    B, S, _ = q_lat.shape
    dt = q_lat.dtype
    q = (rms_norm(q_lat, g_q_a) @ w_q_b).reshape(B, S, MLA_HEADS, QK_NOPE + QK_ROPE)
    q_nope, q_pe = q[..., :QK_NOPE], q[..., QK_NOPE:]
    kv = (rms_norm(kv_lat, g_kv_a) @ w_kv_b).reshape(B, S, MLA_HEADS, QK_NOPE + V_HEAD)
    k_nope, v = kv[..., :QK_NOPE], kv[..., QK_NOPE:]
    inv_freq = 1.0 / (ROPE_THETA ** (jnp.arange(0, QK_ROPE, 2, dtype=jnp.float32) / QK_ROPE))
    ang = positions.astype(jnp.float32)[..., None] * inv_freq
    cos, sin = jnp.cos(ang), jnp.sin(ang)
    q_pe = rotate(q_pe.astype(jnp.float32), cos[:, :, None], sin[:, :, None]).astype(dt)
    k_pe = rotate(k_pe_raw.astype(jnp.float32), cos, sin).astype(dt)
    sm_scale = (QK_NOPE + QK_ROPE) ** -0.5
    chunk_id = np.arange(S) // CHUNK
    outs = []
    for qb in range(S // Q_BLOCK):
        q0, q1 = qb * Q_BLOCK, (qb + 1) * Q_BLOCK
        s = (jnp.einsum('bqhd,bkhd->bhqk', q_nope[:, q0:q1], k_nope[:, :q1])
             + jnp.einsum('bqhr,bkr->bhqk', q_pe[:, q0:q1], k_pe[:, :q1])).astype(jnp.float32) * sm_scale
        mask = chunk_id[q0:q1, None] >= chunk_id[None, :q1]
        s = jnp.where(mask, s, NEG_INF)
        p = jax.nn.softmax(s, axis=-1).astype(dt)
        outs.append(jnp.einsum('bhqk,bkhd->bqhd', p, v[:, :q1]))
    o = jnp.concatenate(outs, axis=1).reshape(B, S, MLA_HEADS * V_HEAD)
    return o @ w_o


def clamped_swiglu(gu):
    gate, up = gu[..., 0::2], gu[..., 1::2]
    gate = jnp.minimum(gate, SWIGLU_LIMIT)
    up = jnp.clip(up, -SWIGLU_LIMIT, SWIGLU_LIMIT)
    return (up + 1) * (gate * jax.nn.sigmoid(SWIGLU_ALPHA * gate))


def moe_ffn(h, w_router, b_router, w_gu, b_gu, w_down, b_down):
    B, S, D = h.shape
    T = B * S
    ht = h.reshape(T, D)
    logits = (ht @ w_router + b_router).astype(jnp.float32)
    top_val, top_idx = lax.top_k(logits, TOP_K)
    weights = jax.nn.softmax(top_val, axis=-1)
    flat_e = top_idx.reshape(-1)
    n_slots = T * TOP_K
    order = jnp.argsort(flat_e, stable=True)
    sorted_e = flat_e[order]
    counts = jnp.bincount(flat_e, length=N_EXPERTS)
    padded = (counts + EXPERT_BLOCK - 1) // EXPERT_BLOCK * EXPERT_BLOCK
    pad_end = jnp.cumsum(padded)
    pad_start = pad_end - padded
    grp_start = jnp.cumsum(counts) - counts
    dest = pad_start[sorted_e] + jnp.arange(n_slots, dtype=jnp.int32) - grp_start[sorted_e]
    n_rows = n_slots + N_EXPERTS * EXPERT_BLOCK
    n_blocks = n_rows // EXPERT_BLOCK
    row_tok = jnp.zeros((n_rows,), jnp.int32).at[dest].set((order // TOP_K).astype(jnp.int32))
    xs = ht[row_tok].reshape(n_blocks, EXPERT_BLOCK, D)
    blk_start = jnp.arange(n_blocks, dtype=jnp.int32) * EXPERT_BLOCK
    blk_e = jnp.minimum(jnp.searchsorted(pad_end, blk_start, side='right'), N_EXPERTS - 1)

    def expert_block(args):
        xb, e = args
        gu = xb @ w_gu[e] + b_gu[e]
        return clamped_swiglu(gu) @ w_down[e] + b_down[e]

    ys = lax.map(expert_block, (xs, blk_e)).reshape(n_rows, D)
    slot_row = jnp.zeros((n_slots,), jnp.int32).at[order].set(dest.astype(jnp.int32))
    y_slots = ys[slot_row].reshape(T, TOP_K, D)
    out = jnp.einsum('tk,tkd->td', weights.astype(ys.dtype), y_slots)
    return out.reshape(B, S, D)


def setup_inputs(seed: int = 0) -> dict:
    key = jax.random.key(seed)
    ks = jax.random.split(key, 32)
    f32 = jnp.float32
    L, D = DEPTH, D_MODEL

    def nrm(k, shape, fan_in, mult=1.0):
        return jax.random.normal(k, shape, f32) * (mult * fan_in ** -0.5)

    def gain(k, shape):
        return 1.0 + 0.1 * jax.random.normal(k, shape, f32)

    def bias(k, shape, s=0.01):
        return s * jax.random.normal(k, shape, f32)

    x = jax.random.normal(ks[0], (BATCH, SEQ, D), f32)
    c = jax.random.normal(ks[1], (BATCH, D), f32)
    offsets = jax.random.randint(ks[2], (BATCH, 1), 0, 8192, dtype=jnp.int32)
    positions = offsets + jnp.arange(SEQ, dtype=jnp.int32)[None, :]
    return {
        'x': x,
        'c': c,
        'positions': positions,
        'w_mod': nrm(ks[3], (L, D, N_MOD * D), D, 0.5),
        'b_mod': bias(ks[4], (L, N_MOD * D), 0.05),
        'g_mix': gain(ks[5], (L, D)),
        'w_in': nrm(ks[6], (L, D, IN_WIDTH), D),
        'b_gate': bias(ks[7], (L, N_BRANCHES * D), 0.1),
        'w_pool_grp': nrm(ks[8], (L, POOL_GROUPS, POOL_GROUP_W, POOL_GROUP_W), POOL_GROUP_W),
        'pool_scale': gain(ks[9], (L, POOL_WIDTH)),
        'w_pool_out': nrm(ks[10], (L, POOL_WIDTH, D), POOL_WIDTH),
        'g_q_a': gain(ks[11], (L, Q_LORA)),
        'w_q_b': nrm(ks[12], (L, Q_LORA, MLA_HEADS * (QK_NOPE + QK_ROPE)), Q_LORA),
        'g_kv_a': gain(ks[13], (L, KV_LORA)),
        'w_kv_b': nrm(ks[14], (L, KV_LORA, MLA_HEADS * (QK_NOPE + V_HEAD)), KV_LORA),
        'w_mla_out': nrm(ks[15], (L, MLA_HEADS * V_HEAD, D), MLA_HEADS * V_HEAD),
        'w_out': nrm(ks[16], (L, D, D), D),
        'g_ffn': gain(ks[17], (L, D)),
        'w_router': nrm(ks[18], (L, D, N_EXPERTS), D),
        'b_router': bias(ks[19], (L, N_EXPERTS)),
        'w_gu': nrm(ks[20], (L, N_EXPERTS, D, 2 * D_EXPERT), D),
        'b_gu': bias(ks[21], (L, N_EXPERTS, 2 * D_EXPERT)),
        'w_down': nrm(ks[22], (L, N_EXPERTS, D_EXPERT, D), D_EXPERT),
        'b_down': bias(ks[23], (L, N_EXPERTS, D)),
        'g_final': gain(ks[24], (D,)),
        'w_fmod': nrm(ks[25], (D, 2 * D), D, 0.5),
        'b_fmod': bias(ks[26], (2 * D,), 0.05),
    }


def reference(x, c, positions, w_mod, b_mod, g_mix, w_in, b_gate, w_pool_grp, pool_scale,
              w_pool_out, g_q_a, w_q_b, g_kv_a, w_kv_b, w_mla_out, w_out, g_ffn,
              w_router, b_router, w_gu, b_gu, w_down, b_down, g_final, w_fmod, b_fmod):
    B, S, D = x.shape
    c_act = jax.nn.silu(c)
    splits = np.cumsum([POOL_WIDTH, Q_LORA, KV_LORA, QK_ROPE]).tolist()
    for l in range(DEPTH):
        mod = (c_act @ w_mod[l] + b_mod[l]).reshape(B, N_MOD, D)
        shift1, scale1, gate1 = mod[:, 0], mod[:, 1], mod[:, 2]
        shift2, scale2, gate2 = mod[:, 3], mod[:, 4], mod[:, 5]

        h = modulate(rms_norm(x, g_mix[l]), shift1, scale1)
        z = h @ w_in[l]
        u, q_lat, kv_lat, k_pe, gate_logits = jnp.split(z, splits, axis=-1)
        a = pool_mixer(u, w_pool_grp[l], pool_scale[l], w_pool_out[l])
        m = mla_mixer(q_lat, kv_lat, k_pe, positions, g_q_a[l], w_q_b[l], g_kv_a[l], w_kv_b[l], w_mla_out[l])
        g = jax.nn.sigmoid((gate_logits + b_gate[l]).astype(jnp.float32)).astype(x.dtype)
        g = g.reshape(B, S, N_BRANCHES, D)
        merged = g[:, :, 0] * a + g[:, :, 1] * m
        x = x + gate1[:, None, :] * (merged @ w_out[l])

        h2 = modulate(rms_norm(x, g_ffn[l]), shift2, scale2)
        x = x + gate2[:, None, :] * moe_ffn(h2, w_router[l], b_router[l], w_gu[l], b_gu[l], w_down[l], b_down[l])

    fmod = c_act @ w_fmod + b_fmod
    fshift, fscale = fmod[:, :D], fmod[:, D:]
    return modulate(rms_norm(x, g_final), fshift, fscale)
```

```python
import numpy as np
from contextlib import ExitStack
import ml_dtypes
import concourse.bass as bass
import concourse.mybir as mybir
from concourse.bass_utils import run_bass_kernel_spmd

F32 = mybir.dt.float32
BF16 = mybir.dt.bfloat16
I32 = mybir.dt.int32
AF = mybir.ActivationFunctionType
ALU = mybir.AluOpType
AX = mybir.AxisListType

S = 4096
D = 1024
NT = 8
TW = 512
NB = 32
NE = 32
EPS = 1e-6
SM_SCALE = 192 ** -0.5
CUT = 99
SKIP = set()
TWO_PI = 6.283185307179586
PI = 3.141592653589793


class Buf:
    __slots__ = ("name", "w", "r", "dsem", "dtot")

    def __init__(self, name):
        self.name = name
        self.w = None
        self.r = []
        self.dsem = None
        self.dtot = 0


class Prog:
    ENG = ("pe", "act", "dve", "pool", "sp")

    def __init__(self, nc, stack):
        self.nc = nc
        self.stack = stack
        self.ops = {e: [] for e in self.ENG}
        self.sem = {e: stack.enter_context(nc.semaphore("s_" + e)) for e in self.ENG}
        self.tick = {e: 0 for e in self.ENG}
        self.seen = {e: {} for e in self.ENG}
        self.semobj = {}
        self.dbufs = []
        self.free = []
        for e in self.ENG:
            self.semobj[("eng", e)] = self.sem[e]
        self.nd = 0

    def _need(self, eng, toks):
        best = {}
        for t in toks:
            if t is None:
                continue
            k, v = t
            if k == ("eng", eng) and eng == "pe":
                continue
            if best.get(k, 0) < v:
                best[k] = v
        waits = []
        for k, v in best.items():
            if self.seen[eng].get(k, 0) >= v:
                continue
            self.seen[eng][k] = v
            waits.append((self.semobj[k], v))
        return waits

    @staticmethod
    def _deps(reads, writes):
        toks = []
        for b in reads:
            toks.append(b.w)
        for b in writes:
            toks.append(b.w)
            toks.extend(b.r)
        return toks

    @staticmethod
    def _mark(tok, reads, writes):
        for b in reads:
            b.r.append(tok)
        for b in writes:
            b.w = tok
            b.r = []

    def op(self, eng, fn, reads=(), writes=()):
        waits = self._need(eng, self._deps(reads, writes))
        self.tick[eng] += 1
        tok = (("eng", eng), self.tick[eng])
        sem = self.sem[eng]

        def run(e, fn=fn, waits=waits, sem=sem):
            for s_, v in waits:
                e.wait_ge(s_, v)
            fn(e).then_inc(sem, 1)
        self.ops[eng].append(run)
        self._mark(tok, reads, writes)
        return tok

    def dma(self, q, fn, sb, reads=(), writes=()):
        if sb.dsem is None:
            if self.free:
                sb.dsem = self.free.pop()
            else:
                self.nd += 1
                h = self.stack.enter_context(self.nc.semaphore("d%d" % self.nd))
                sb.dsem = [h, 0, self.nd]
                self.semobj[("dma", self.nd)] = h
            self.dbufs.append(sb)
        ds = sb.dsem
        key = ("dma", ds[2])
        toks = self._deps(reads, writes)
        if ds[1] > 0:
            toks.append((key, ds[1]))
        waits = self._need(q, toks)
        ds[1] += 16
        tok = (key, ds[1])
        sem = ds[0]

        def run(e, fn=fn, waits=waits, sem=sem):
            for s_, v in waits:
                e.wait_ge(s_, v)
            fn(e).then_inc(sem, 16)
        self.ops[q].append(run)
        self._mark(tok, reads, writes)
        return tok

    def barrier(self):
        toks = [(("eng", e), self.tick[e]) for e in self.ENG if self.tick[e] > 0]
        toks += [(("dma", b.dsem[2]), b.dsem[1]) for b in self.dbufs]
        for e in self.ENG:
            waits = self._need(e, toks)

            def run(en, waits=waits):
                for s_, v in waits:
                    en.wait_ge(s_, v)
            self.ops[e].append(run)
        for b in self.dbufs:
            self.free.append(b.dsem)
            b.dsem = None
        self.dbufs = []

    def emit(self):
        nc = self.nc
        ops = self.ops
        with nc.Block() as block:
            @block.tensor
            def _(e):
                for f in ops["pe"]:
                    f(e)

            @block.scalar
            def _(e):
                for f in ops["act"]:
                    f(e)

            @block.vector
            def _(e):
                for f in ops["dve"]:
                    f(e)

            @block.gpsimd
            def _(e):
                for f in ops["pool"]:
                    f(e)

            @block.sync
            def _(e):
                for f in ops["sp"]:
                    f(e)
        self.ops = {e: [] for e in self.ENG}


class T:
    def __init__(self, t, name):
        self.t = t
        self.b = Buf(name)

    def __getitem__(self, idx):
        return self.t[idx]


IN_SPECS = [
    ("x", [S, D], F32), ("cT", [128, 8], F32), ("pos", [1, S], I32),
    ("w_mod", [D, 6 * D], F32), ("b_mod", [1, 6 * D], F32), ("g_mix", [128, 8], F32),
    ("w_in", [D, 4160], F32), ("b_gate", [128, 16], F32),
    ("w_grp", [4, 256, 256], F32), ("pscale", [128, 8], F32), ("w_proj", [D, D], F32),
    ("g_q", [128, 6], F32), ("w_q", [768, 1536], F32), ("g_kv", [128, 2], F32), ("w_kv", [256, 2048], F32),
    ("w_mo", [D, D], F32), ("w_out", [D, D], F32), ("g_ffn", [1, D], F32),
    ("w_r", [D, NE], F32), ("b_r", [1, NE], F32),
    ("w_gu", [NE, D, 2 * D], F32), ("b_gu", [128, NE * 16], F32), ("w_dn", [NE, D, D], F32), ("b_dn", [NE, D], F32),
    ("g_fin", [1, D], F32), ("w_fmod", [D, 2 * D], F32), ("b_fmod", [1, 2 * D], F32),
    ("identb", [128, 128], BF16), ("identf", [128, 128], F32), ("rmat", [64, 64], BF16),
    ("invf", [128, 1], F32), ("rc0", [1, 64], F32),
    ("utri", [128, 128], BF16), ("bcol", [128, 2], F32), ("par", [1, 2 * NE], F32), ("b_gu_r", [NE, 2 * D], F32),
]

SCRATCH = [
    ("cos_d", [64, S], F32), ("sin_d", [64, S], F32),
    ("h_d", [NT, 128, 8, TW], BF16), ("a_d", [NT, 128, 8, TW], BF16),
    ("qn_d", [8, 128, S], BF16), ("qp_d", [8, 64, S], BF16),
    ("kn_d", [8, 128, S], BF16), ("kp_d", [64, S], BF16), ("v_d", [NB, 128, D], BF16),
    ("o_d", [NT, 128, 8, TW], BF16), ("x1_d", [NB, 128, D], F32), ("h2_d", [NT, 128, 8, TW], BF16),
    ("wd_d", [128, NB * NE], F32), ("mod_d", [128, 3 * D], F32),
    ("h2tok_d", [S, D], BF16), ("xs_d", [160 * 128, D], BF16), ("ys_d", [160 * 128, D], F32), ("tbl_d", [128, 168], I32),
]


def build(dbg=(), stop_after=None):
    nc = bass.Bass("TRN2", target_bir_lowering=False)
    dr = {}
    for n, shp, dt in IN_SPECS:
        dr[n] = nc.dram_tensor(n, shp, dt, kind="ExternalInput").ap()
    for n, shp, dt in SCRATCH:
        kind = "ExternalOutput" if n in dbg else "Internal"
        dr[n] = nc.dram_tensor(n, shp, dt, kind=kind).ap()
    dr["out"] = nc.dram_tensor("out", [S, D], F32, kind="ExternalOutput").ap()
    db = {}
    for n in ("cos_d", "sin_d", "kp_d", "wd_d", "mod_d"):
        db[n] = Buf(n)
    for n in ("h_d", "a_d", "o_d", "h2_d"):
        db[n] = [Buf(n + str(i)) for i in range(NT)]
    for n in ("qn_d", "qp_d", "kn_d"):
        db[n] = [Buf(n + str(i)) for i in range(NT)]
    db["h2tok_d"] = [Buf("h2tok" + str(i)) for i in range(NB)]
    for n in ("v_d", "x1_d"):
        db[n] = [Buf(n + str(i)) for i in range(NB)]
    db["out"] = [Buf("out" + str(i)) for i in range(NB)]

    with ExitStack() as G:
        P = Prog(nc, G)

        def sb(st, name, shape, dt):
            return T(st.enter_context(nc.sbuf_tensor("s_" + name, shape, dt)), name)

        def ps(st, name, shape, dt):
            return T(st.enter_context(nc.psum_tensor("p_" + name, shape, dt)), name)

        def load(q, dst, dst_ap, src_ap, srcbufs=(), **kw):
            P.dma(q, lambda e: e.dma_start(out=dst_ap, in_=src_ap, **kw), dst.b, reads=list(srcbufs), writes=[dst.b])

        def store(q, src, dst_ap, src_ap, dstbufs=()):
            P.dma(q, lambda e: e.dma_start(out=dst_ap, in_=src_ap), src.b, reads=[src.b], writes=list(dstbufs))

        identb = sb(G, "identb", [128, 128], BF16)
        identf = sb(G, "identf", [128, 128], F32)
        onesf = sb(G, "onesf", [128, 128], F32)
        onesb = sb(G, "onesb", [128, 128], BF16)
        lateB = sb(G, "lateB", [128, 3, D], F32)
        wdense = sb(G, "wdense", [128, NB, NE], F32)
        bguc = sb(G, "bguc", [128, NE * 16], F32)
        load("sp", identb, identb[:], dr["identb"][:, :])
        load("sp", identf, identf[:], dr["identf"][:, :])
        load("sp", bguc, bguc[:], dr["b_gu"][:, :])
        P.op("pool", lambda e: e.memset(onesf[:], 1.0), writes=[onesf.b])
        P.op("pool", lambda e: e.memset(onesb[:], 1.0), writes=[onesb.b])

        with ExitStack() as S1:
            A1c = sb(S1, "A1c", [128, 8], F32)
            sh1c = sb(S1, "sh1c", [128, 8], F32)

            with ExitStack() as SA:
                wina = sb(SA, "wina", [128, 8, 2112], BF16)
                wgrp = sb(SA, "wgrp", [128, 4, 2, 256], BF16)
                wproj = sb(SA, "wproj", [128, 8, D], BF16)
                wq = sb(SA, "wq", [128, 6, 1536], BF16)
                wkv = sb(SA, "wkv", [128, 2, 2048], BF16)
                for k in range(8):
                    load("pool", wina, wina[:, k, :], dr["w_in"][k * 128:(k + 1) * 128, 0:2112], max_dma_last_dim=4096)
                load("pool", wgrp, wgrp[:], dr["w_grp"].rearrange("g (ki p) d -> p g ki d", p=128))
                for k in range(8):
                    load("pool", wproj, wproj[:, k, :], dr["w_proj"][k * 128:(k + 1) * 128, :])
                for k in range(6):
                    load("pool", wq, wq[:, k, :], dr["w_q"][k * 128:(k + 1) * 128, :], max_dma_last_dim=4096)
                for k in range(2):
                    load("pool", wkv, wkv[:, k, :], dr["w_kv"][k * 128:(k + 1) * 128, :], max_dma_last_dim=4096)
                with ExitStack() as S0:
                    modB = sb(S0, "modB", [128, 6 * D], F32)
                    fmodB = sb(S0, "fmodB", [128, 2 * D], F32)
                    csb = sb(S0, "csb", [128, 8], F32)
                    cact = sb(S0, "cact", [128, 8], F32)
                    crep = sb(S0, "crep", [128, 8, 128], F32)
                    gmc = sb(S0, "gmc", [128, 8], F32)
                    gffB = sb(S0, "gffB", [128, D], F32)
                    gfinB = sb(S0, "gfinB", [128, D], F32)
                    wt = [sb(S0, "wt%d" % i, [128, 8, 512], F32) for i in range(2)]
                    pm = [ps(S0, "pm%d" % i, [128, 512], F32) for i in range(2)]
                    dtmp = sb(S0, "dtmp", [128, 128], F32)
                    dcol = sb(S0, "dcol", [128, 16], F32)
                    load("sp", csb, csb[:], dr["cT"][:, :])
                    load("sp", gmc, gmc[:], dr["g_mix"][:, :])
                    load("sp", modB, modB[:], dr["b_mod"][0].partition_broadcast(128))
                    load("sp", fmodB, fmodB[:], dr["b_fmod"][0].partition_broadcast(128))
                    load("sp", gffB, gffB[:], dr["g_ffn"][0].partition_broadcast(128))
                    load("sp", gfinB, gfinB[:], dr["g_fin"][0].partition_broadcast(128))
                    P.op("act", lambda e: e.activation(out=cact[:], in_=csb[:], func=AF.Silu), reads=[csb.b], writes=[cact.b])

                    def mk_crep(e):
                        for k in range(8):
                            i = e.tensor_scalar(out=crep[:, k, :], in0=onesf[:], scalar1=cact[:, k:k + 1], scalar2=None, op0=ALU.mult)
                        return i
                    P.op("dve", mk_crep, reads=[onesf.b, cact.b], writes=[crep.b])
                    for ci in range(16):
                        w_src = dr["w_mod"] if ci < 12 else dr["w_fmod"]
                        c0 = (ci if ci < 12 else ci - 12) * 512
                        tgt = modB if ci < 12 else fmodB
                        wb = wt[ci % 2]
                        pb = pm[ci % 2]
                        load("sp", wb, wb[:], w_src.rearrange("(k p) n -> p k n", p=128)[:, :, c0:c0 + 512])

                        def mm(e, wb=wb, pb=pb):
                            for k in range(8):
                                i = e.matmul(pb[:], lhsT=crep[:, k, :], rhs=wb[:, k, :], start=(k == 0), stop=(k == 7))
                            return i
                        P.op("pe", mm, reads=[crep.b, wb.b], writes=[pb.b])
                        P.op("dve", lambda e, pb=pb, tgt=tgt, c0=c0: e.tensor_tensor(
                            out=tgt[:, c0:c0 + 512], in0=pb[:], in1=tgt[:, c0:c0 + 512], op=ALU.add),
                            reads=[pb.b, tgt.b], writes=[tgt.b])
                    for j in range(16):
                        c0 = j * 128
                        P.op("dve", lambda e, c0=c0: e.tensor_tensor(out=dtmp[:], in0=modB[:, c0:c0 + 128], in1=identf[:], op=ALU.mult),
                             reads=[modB.b, identf.b], writes=[dtmp.b])
                        P.op("dve", lambda e, j=j: e.reduce_sum(out=dcol[:, j:j + 1], in_=dtmp[:], axis=AX.X),
                             reads=[dtmp.b], writes=[dcol.b])
                    P.op("dve", lambda e: e.tensor_copy(out=sh1c[:], in_=dcol[:, 0:8]), reads=[dcol.b], writes=[sh1c.b])
                    P.op("dve", lambda e: e.scalar_tensor_tensor(out=A1c[:], in0=dcol[:, 8:16], scalar=1.0, in1=gmc[:], op0=ALU.add, op1=ALU.mult),
                         reads=[dcol.b, gmc.b], writes=[A1c.b])
                    P.op("dve", lambda e: e.scalar_tensor_tensor(out=modB[:, 4 * D:5 * D], in0=modB[:, 4 * D:5 * D], scalar=1.0, in1=gffB[:],
                                                                 op0=ALU.add, op1=ALU.mult), reads=[modB.b, gffB.b], writes=[modB.b])
                    P.op("pool", lambda e: e.tensor_copy(out=lateB[:, 0, :], in_=modB[:, 5 * D:6 * D]), reads=[modB.b], writes=[lateB.b])
                    P.op("dve", lambda e: e.scalar_tensor_tensor(out=lateB[:, 1, :], in0=fmodB[:, D:2 * D], scalar=1.0, in1=gfinB[:],
                                                                 op0=ALU.add, op1=ALU.mult), reads=[fmodB.b, gfinB.b], writes=[lateB.b])
                    P.op("pool", lambda e: e.tensor_copy(out=lateB[:, 2, :], in_=fmodB[:, 0:D]), reads=[fmodB.b], writes=[lateB.b])

                    store("pool", modB, dr["mod_d"][:, :], modB[:, 2 * D:5 * D], [db["mod_d"]])
                    posi = sb(S0, "posi", [128, S // 4], I32)
                    ang = sb(S0, "ang", [128, S // 4], F32)
                    kf = sb(S0, "kf", [128, S // 4], F32)
                    ki = sb(S0, "ki", [128, S // 4], I32)
                    invf = sb(S0, "invf", [128, 1], F32)
                    for q in range(4):
                        load("sp", posi, posi[q * 32:(q + 1) * 32, :], dr["pos"][0, q * (S // 4):(q + 1) * (S // 4)].partition_broadcast(32))
                    load("sp", invf, invf[:], dr["invf"][:, :])
                    P.op("dve", lambda e: e.tensor_copy(out=kf[:], in_=posi[:]), reads=[posi.b], writes=[kf.b])
                    P.op("dve", lambda e: e.tensor_scalar(out=ang[:], in0=kf[:], scalar1=invf[:, 0:1], scalar2=None, op0=ALU.mult),
                         reads=[kf.b, invf.b], writes=[ang.b])
                    for which, shift in (("sin_d", 0.0), ("cos_d", PI / 2)):
                        if shift != 0.0:
                            P.op("dve", lambda e, shift=shift: e.tensor_scalar(out=ang[:], in0=ang[:], scalar1=shift, scalar2=None, op0=ALU.add),
                                 reads=[ang.b], writes=[ang.b])
                        P.op("dve", lambda e: e.tensor_scalar(out=kf[:], in0=ang[:], scalar1=1.0 / TWO_PI, scalar2=None, op0=ALU.mult),
                             reads=[ang.b], writes=[kf.b])
                        P.op("dve", lambda e: e.tensor_copy(out=ki[:], in_=kf[:]), reads=[kf.b], writes=[ki.b])
                        P.op("dve", lambda e: e.tensor_copy(out=kf[:], in_=ki[:]), reads=[ki.b], writes=[kf.b])
                        P.op("dve", lambda e: e.scalar_tensor_tensor(out=kf[:], in0=kf[:], scalar=-TWO_PI, in1=ang[:], op0=ALU.mult, op1=ALU.add),
                             reads=[kf.b, ang.b], writes=[kf.b])
                        P.op("dve", lambda e: e.tensor_scalar(out=posi[:].bitcast(F32), in0=kf[:], scalar1=PI, scalar2=TWO_PI, op0=ALU.is_gt, op1=ALU.mult),
                             reads=[kf.b], writes=[posi.b])
                        P.op("dve", lambda e: e.tensor_tensor(out=kf[:], in0=kf[:], in1=posi[:].bitcast(F32), op=ALU.subtract),
                             reads=[kf.b, posi.b], writes=[kf.b])
                        P.op("dve", lambda e: e.tensor_scalar(out=posi[:].bitcast(F32), in0=kf[:], scalar1=-PI, scalar2=TWO_PI, op0=ALU.is_lt, op1=ALU.mult),
                             reads=[kf.b], writes=[posi.b])
                        P.op("dve", lambda e: e.tensor_tensor(out=kf[:], in0=kf[:], in1=posi[:].bitcast(F32), op=ALU.add),
                             reads=[kf.b, posi.b], writes=[kf.b])
                        P.op("dve", lambda e: e.tensor_scalar(out=kf[:], in0=kf[:], scalar1=3.1415925, scalar2=-3.1415925, op0=ALU.min, op1=ALU.max),
                             reads=[kf.b], writes=[kf.b])
                        P.op("act", lambda e: e.activation(out=kf[:], in_=kf[:], func=AF.Sin), reads=[kf.b], writes=[kf.b])
                        for q in range(4):
                            for dup in range(2):
                                P.dma("pool", lambda e, which=which, q=q, dup=dup: e.dma_start(
                                    out=dr[which][dup * 32:(dup + 1) * 32, q * (S // 4):(q + 1) * (S // 4)], in_=kf[q * 32:(q + 1) * 32, :]),
                                    Buf("rst"), reads=[kf.b], writes=[db[which]])
                    P.barrier()
                    P.emit()
                if stop_after == 0:
                    return nc

                rmat = sb(SA, "rmat", [64, 64], BF16)
                rc0 = sb(SA, "rc0", [128, 64], F32)
                load("sp", rc0, rc0[:], dr["rc0"][0].partition_broadcast(128))
                ptmp = sb(SA, "ptmp", [128, 16], F32)
                psc = sb(SA, "psc", [128, 8], F32)
                gqc = sb(SA, "gqc", [128, 6], F32)
                gkc = sb(SA, "gkc", [128, 2], F32)
                load("sp", rmat, rmat[:], dr["rmat"][:, :])
                load("sp", psc, psc[:], dr["pscale"][:, :])
                load("sp", gqc, gqc[:], dr["g_q"][:, :])
                load("sp", gkc, gkc[:], dr["g_kv"][:, :])

                xt = [sb(SA, "xt%d" % i, [128, D], F32) for i in range(2)]
                xn = [sb(SA, "xn%d" % i, [128, D], BF16) for i in range(2)]
                ssq = [sb(SA, "ssq%d" % i, [128, 1], F32) for i in range(2)]
                hT = [sb(SA, "hT%d" % i, [128, 8, TW], BF16) for i in range(2)]
                uext = [sb(SA, "uext%d" % i, [128, 528], F32) for i in range(2)]
                s2 = sb(SA, "s2", [128, 528], F32)
                s4 = sb(SA, "s4", [128, 528], F32)
                s8 = sb(SA, "s8", [128, 528], F32)
                s16 = sb(SA, "s16", [128, 528], F32)
                halo = [sb(SA, "halo%d" % i, [128, 16], F32) for i in range(8)]
                mixed = sb(SA, "mixed", [128, 8, TW], BF16)
                yT = sb(SA, "yT", [128, 8, TW], BF16)
                aT = [sb(SA, "aT%d" % i, [128, TW], BF16) for i in range(2)]
                zq = sb(SA, "zq", [128, 6, TW], BF16)
                sqb = [sb(SA, "sqb%d" % i, [128, TW], BF16) for i in range(2)]
                rq = sb(SA, "rq", [128, TW], F32)
                qnT = sb(SA, "qnT", [128, 6, TW], BF16)
                qo = [sb(SA, "qo%d" % i, [128, TW], BF16) for i in range(2)]
                qpo = [sb(SA, "qpo%d" % i, [64, TW], BF16) for i in range(2)]
                ko = [sb(SA, "ko%d" % i, [128, TW], BF16) for i in range(2)]
                kpo = [sb(SA, "kpo%d" % i, [64, TW], BF16) for i in range(2)]
                vo = [sb(SA, "vo%d" % i, [128, D], BF16) for i in range(2)]
                abf = sb(SA, "abf", [64, TW], BF16)
                rt1 = sb(SA, "rt1", [64, TW], F32)
                rt2 = sb(SA, "rt2", [64, TW], F32)
                cosT = sb(SA, "cosT", [64, TW], F32)
                sinT = sb(SA, "sinT", [64, TW], F32)
                zkv = sb(SA, "zkv", [128, 2, TW], BF16)
                rkv = sb(SA, "rkv", [128, TW], F32)
                kvn = sb(SA, "kvn", [128, 2, TW], BF16)
                ptr = [ps(SA, "ptr%d" % i, [128, 8, 128], BF16) for i in range(2)]
                zps = [ps(SA, "zps%d" % i, [128, TW], F32) for i in range(4)]
                ssps = ps(SA, "ssps", [128, TW], F32)
                rps = ps(SA, "rps", [128, TW], F32)
                zi = [0]

                def nz():
                    zi[0] += 1
                    return zps[zi[0] % 4]

                for c in range(8):
                    P.op("pool", lambda e, c=c: e.memset(halo[c][:], 0.0), writes=[halo[c].b])

                def rstd_from(pssum, dst, n):
                    P.op("dve", lambda e: e.tensor_scalar(out=dst[:], in0=pssum[:], scalar1=1.0 / n, scalar2=EPS, op0=ALU.mult, op1=ALU.add),
                         reads=[pssum.b], writes=[dst.b])
                    P.op("act", lambda e: e.activation(out=dst[:], in_=dst[:], func=AF.Sqrt), reads=[dst.b], writes=[dst.b])
                    P.op("dve", lambda e: e.reciprocal(out=dst[:], in_=dst[:]), reads=[dst.b], writes=[dst.b])

                def rotary(pz, dst_ap, dstT, rows=64):
                    P.op("act", lambda e: e.copy(out=abf[:], in_=pz[0:64, :]), reads=[pz.b], writes=[abf.b])
                    P.op("pe", lambda e: e.matmul(rps[0:64, :], lhsT=rmat[:], rhs=abf[:], start=True, stop=True),
                         reads=[rmat.b, abf.b], writes=[rps.b])
                    P.op("dve", lambda e: e.tensor_tensor(out=rt1[:], in0=pz[0:64, :], in1=cosT[:], op=ALU.mult),
                         reads=[pz.b, cosT.b, abf.b], writes=[rt1.b])
                    P.op("dve", lambda e: e.tensor_tensor(out=rt2[:], in0=rps[0:64, :], in1=sinT[:], op=ALU.mult),
                         reads=[rps.b, sinT.b], writes=[rt2.b])
                    P.op("pool", lambda e: e.tensor_tensor(out=dst_ap, in0=rt1[:], in1=rt2[:], op=ALU.add),
                         reads=[rt1.b, rt2.b], writes=[dstT.b])

                def front(Tt):
                    hb = hT[Tt % 2]
                    for blk in range(4):
                        i = Tt * 4 + blk
                        xb_, xnb, sq_, pt_ = xt[i % 2], xn[i % 2], ssq[i % 2], ptr[i % 2]
                        load("sp", xb_, xb_[:], dr["x"][i * 128:(i + 1) * 128, :])
                        P.op("act", lambda e, xb_=xb_, sq_=sq_, xnb=xnb: e.activation(out=xnb[:], in_=xb_[:], func=AF.Square, accum_out=sq_[:]),
                             reads=[xb_.b], writes=[xnb.b, sq_.b])
                        rstd_from(sq_, sq_, D)
                        P.op("dve", lambda e, xb_=xb_, xnb=xnb, sq_=sq_: e.tensor_scalar(out=xnb[:], in0=xb_[:], scalar1=sq_[:, 0:1], scalar2=None, op0=ALU.mult),
                             reads=[xb_.b, sq_.b], writes=[xnb.b])

                        def tr(e, xnb=xnb, pt_=pt_):
                            for k in range(8):
                                ins = e.transpose(out=pt_[:, k, :], in_=xnb[:, k * 128:(k + 1) * 128], identity=identb[:])
                            return ins
                        P.op("pe", tr, reads=[xnb.b, identb.b], writes=[pt_.b])

                        def ev(e, pt_=pt_, hb=hb, blk=blk, ks=(0, 1, 2, 3)):
                            for k in ks:
                                ins = e.activation(out=hb[:, k, blk * 128:(blk + 1) * 128], in_=pt_[:, k, :], func=AF.Identity,
                                                   scale=A1c[:, k:k + 1], bias=sh1c[:, k:k + 1])
                            return ins

                        def ev2(e, pt_=pt_, hb=hb, blk=blk, ks=(4, 5, 6, 7)):
                            for k in ks:
                                ins = e.tensor_scalar(out=hb[:, k, blk * 128:(blk + 1) * 128], in0=pt_[:, k, :],
                                                      scalar1=A1c[:, k:k + 1], scalar2=sh1c[:, k:k + 1], op0=ALU.mult, op1=ALU.add)
                            return ins
                        if "ev" not in SKIP:
                            P.op("act", ev, reads=[pt_.b, A1c.b, sh1c.b], writes=[hb.b])
                        if "ev2" not in SKIP:
                            P.op("dve", ev2, reads=[pt_.b, A1c.b, sh1c.b], writes=[hb.b])
                    store("sp", hb, dr["h_d"][Tt], hb[:], [db["h_d"][Tt]])

                front(0)
                for Tt in range(NT):
                    t0 = Tt * TW
                    hb = hT[Tt % 2]
                    load("sp", cosT, cosT[:], dr["cos_d"][:, t0:t0 + TW], [db["cos_d"]])
                    load("sp", sinT, sinT[:], dr["sin_d"][:, t0:t0 + TW], [db["sin_d"]])

                    def zmm(pz, c0, m=128, hb=hb):
                        def f(e, hb=hb):
                            for k in range(8):
                                ins = e.matmul(pz[0:m, :], lhsT=wina[:, k, c0:c0 + m], rhs=hb[:, k, :], start=(k == 0), stop=(k == 7))
                            return ins
                        P.op("pe", f, reads=[wina.b, hb.b], writes=[pz.b])

                    for c in range(8):
                        g = c // 2
                        ue = uext[c % 2]
                        pz = nz()
                        zmm(pz, c * 128)
                        P.op("pool", lambda e, ue=ue, c=c: e.tensor_copy(out=ue[:, 1:16], in_=halo[c][:, 1:16]), reads=[halo[c].b], writes=[ue.b])
                        P.op("act", lambda e, ue=ue, pz=pz: e.copy(out=ue[:, 16:528], in_=pz[:]), reads=[pz.b], writes=[ue.b])
                        P.op("pool", lambda e, ue=ue, c=c: e.tensor_copy(out=halo[c][:, 1:16], in_=ue[:, 513:528]), reads=[ue.b], writes=[halo[c].b])
                        chain = [(s2, 2, 1), (s4, 4, 2), (s8, 8, 4), (s16, 16, 8)][:g + 1]
                        prev = ue
                        for (sx, lo, sh) in chain:
                            P.op("dve", lambda e, sx=sx, lo=lo, sh=sh, prev=prev: e.tensor_tensor(
                                out=sx[:, lo:528], in0=prev[:, lo:528], in1=prev[:, lo - sh:528 - sh], op=ALU.add),
                                reads=[prev.b], writes=[sx.b])
                            prev = sx
                        w = 2 ** (g + 1)
                        P.op("dve", lambda e, prev=prev, ue=ue, c=c, w=w: e.scalar_tensor_tensor(
                            out=mixed[:, c, :], in0=prev[:, 16:528], scalar=1.0 / w, in1=ue[:, 16:528], op0=ALU.mult, op1=ALU.subtract),
                            reads=[prev.b, ue.b], writes=[mixed.b])
                        if Tt == 0:
                            P.op("dve", lambda e, prev=prev, g=g: e.tensor_tensor(out=ptmp[:], in0=prev[:, 16:32], in1=rc0[:, g * 16:(g + 1) * 16], op=ALU.mult),
                                 reads=[prev.b, rc0.b], writes=[ptmp.b])
                            P.op("dve", lambda e, ue=ue, c=c: e.tensor_tensor(out=mixed[:, c, 0:16], in0=ptmp[:], in1=ue[:, 16:32], op=ALU.subtract),
                                 reads=[ptmp.b, ue.b], writes=[mixed.b])
                    def latent1(nch, col0, ztile, gcol, pss):
                        for c in range(nch):
                            pz = nz()
                            zmm(pz, col0 + c * 128)
                            sq_ = sqb[c % 2]
                            P.op("act", lambda e, pz=pz, sq_=sq_: e.activation(out=sq_[:], in_=pz[:], func=AF.Square), reads=[pz.b], writes=[sq_.b])
                            P.op("act", lambda e, pz=pz, c=c: e.activation(out=ztile[:, c, :], in_=pz[:], func=AF.Identity, scale=gcol[:, c:c + 1]),
                                 reads=[pz.b, gcol.b], writes=[ztile.b])
                            P.op("pe", lambda e, sq_=sq_, c=c: e.matmul(pss[:], lhsT=onesb[:], rhs=sq_[:], start=(c == 0), stop=(c == nch - 1)),
                                 reads=[onesb.b, sq_.b], writes=[pss.b])

                    def latent2(nch, ztile, rdst, ndst, nfeat, pss):
                        rstd_from(pss, rdst, nfeat)
                        for c in range(nch):
                            P.op("dve", lambda e, c=c: e.tensor_tensor(out=ndst[:, c, :], in0=ztile[:, c, :], in1=rdst[:], op=ALU.mult),
                                 reads=[ztile.b, rdst.b], writes=[ndst.b])
                    latent1(6, 1024, zq, gqc, ssps)
                    latent1(2, 1792, zkv, gkc, rps)
                    latent2(6, zq, rq, qnT, 768, ssps)
                    latent2(2, zkv, rkv, kvn, 256, rps)
                    for g in range(4):
                        for mo in range(2):
                            pz = nz()

                            def f(e, pz=pz, g=g, mo=mo):
                                for kk in range(2):
                                    ins = e.matmul(pz[:], lhsT=wgrp[:, g, kk, mo * 128:(mo + 1) * 128], rhs=mixed[:, 2 * g + kk, :],
                                                   start=(kk == 0), stop=(kk == 1))
                                return ins
                            P.op("pe", f, reads=[wgrp.b, mixed.b], writes=[pz.b])
                            cc = 2 * g + mo
                            P.op("act", lambda e, pz=pz, cc=cc: e.activation(out=yT[:, cc, :], in_=pz[:], func=AF.Identity, scale=psc[:, cc:cc + 1]),
                                 reads=[pz.b, psc.b], writes=[yT.b])
                    for mo in range(8):
                        pz = nz()
                        ab = aT[mo % 2]

                        def f(e, pz=pz, mo=mo):
                            for k in range(8):
                                ins = e.matmul(pz[:], lhsT=wproj[:, k, mo * 128:(mo + 1) * 128], rhs=yT[:, k, :], start=(k == 0), stop=(k == 7))
                            return ins
                        P.op("pe", f, reads=[wproj.b, yT.b], writes=[pz.b])
                        eng = "act" if mo % 2 == 0 else "dve"
                        if eng == "act":
                            P.op("act", lambda e, pz=pz, mo=mo, ab=ab: e.copy(out=ab[:], in_=pz[:]), reads=[pz.b], writes=[ab.b])
                        else:
                            P.op("dve", lambda e, pz=pz, mo=mo, ab=ab: e.tensor_copy(out=ab[:], in_=pz[:]), reads=[pz.b], writes=[ab.b])
                        store("sp", ab, dr["a_d"][Tt][:, mo, :], ab[:], [db["a_d"][Tt]])

                    if Tt + 1 < NT:
                        front(Tt + 1)
                    for h in range(8):
                        pz = nz()
                        qob = qo[h % 2]

                        def f(e, pz=pz, h=h):
                            for k in range(6):
                                ins = e.matmul(pz[:], lhsT=wq[:, k, h * 128:(h + 1) * 128], rhs=qnT[:, k, :], start=(k == 0), stop=(k == 5))
                            return ins
                        P.op("pe", f, reads=[wq.b, qnT.b], writes=[pz.b])
                        P.op("act", lambda e, pz=pz, h=h, qob=qob: e.copy(out=qob[:], in_=pz[:]), reads=[pz.b], writes=[qob.b])
                        store("sp", qob, dr["qn_d"][h, :, t0:t0 + TW], qob[:], [db["qn_d"][Tt]])
                    for h in range(8):
                        pz = nz()
                        qpb = qpo[h % 2]

                        def f(e, pz=pz, h=h):
                            for k in range(6):
                                ins = e.matmul(pz[0:64, :], lhsT=wq[:, k, 1024 + h * 64:1024 + (h + 1) * 64], rhs=qnT[:, k, :], start=(k == 0), stop=(k == 5))
                            return ins
                        P.op("pe", f, reads=[wq.b, qnT.b], writes=[pz.b])
                        rotary(pz, qpb[:], qpb)
                        store("sp", qpb, dr["qp_d"][h, :, t0:t0 + TW], qpb[:], [db["qp_d"][Tt]])

                    kpb = kpo[Tt % 2]
                    for h in range(8):
                        pz = nz()
                        kob = ko[h % 2]

                        def f(e, pz=pz, h=h):
                            for k in range(2):
                                ins = e.matmul(pz[:], lhsT=wkv[:, k, h * 128:(h + 1) * 128], rhs=kvn[:, k, :], start=(k == 0), stop=(k == 1))
                            return ins
                        P.op("pe", f, reads=[wkv.b, kvn.b], writes=[pz.b])
                        P.op("dve", lambda e, pz=pz, h=h, kob=kob: e.tensor_copy(out=kob[:], in_=pz[:]), reads=[pz.b], writes=[kob.b])
                        store("sp", kob, dr["kn_d"][h, :, t0:t0 + TW], kob[:], [db["kn_d"][Tt]])
                    for blk in range(4):
                        i = Tt * 4 + blk
                        vb = vo[i % 2]
                        for j in range(2):
                            pz = nz()

                            def f(e, pz=pz, blk=blk, j=j):
                                for k in range(2):
                                    ins = e.matmul(pz[:], lhsT=kvn[:, k, blk * 128:(blk + 1) * 128], rhs=wkv[:, k, 1024 + j * 512:1024 + (j + 1) * 512],
                                                   start=(k == 0), stop=(k == 1))
                                return ins
                            P.op("pe", f, reads=[wkv.b, kvn.b], writes=[pz.b])
                            P.op("act", lambda e, pz=pz, j=j, vb=vb: e.copy(out=vb[:, j * 512:(j + 1) * 512], in_=pz[:]), reads=[pz.b], writes=[vb.b])
                        store("sp", vb, dr["v_d"][i], vb[:], [db["v_d"][i]])
                    pz = nz()
                    zmm(pz, 2048, 64)
                    rotary(pz, kpb[:], kpb)
                    store("sp", kpb, dr["kp_d"][:, t0:t0 + TW], kpb[:], [db["kp_d"]])
                P.barrier()
                P.emit()
            if stop_after == 1:
                return nc

            with ExitStack() as SB:
                kc = sb(SB, "kc", [128, 8, S], BF16)
                kpc = sb(SB, "kpc", [128, S], BF16)
                vaug = sb(SB, "vaug", [128, NB, 8, 129], BF16)
                qt = [sb(SB, "qt0", [128, 8, TW], BF16)] * 2
                qpt = [sb(SB, "qpt0", [128, 8, TW], BF16)] * 2
                pT = [sb(SB, "pT%d" % i, [128, TW], BF16) for i in range(3)]
                otok = sb(SB, "otok", [128, 4, D], BF16)
                oT = [sb(SB, "oT0", [128, 8, TW], BF16)] * 2
                rs = sb(SB, "rs", [128, 4], F32)
                sps = [ps(SB, "sps%d" % i, [128, TW], F32) for i in range(3)]
                ops_ = [ps(SB, "ops%d" % i, [128, 512], F32) for i in range(4)]
                pto = ps(SB, "pto", [128, 8, 128], BF16)
                kcb = [Buf("kc%d" % i) for i in range(NT)]
                vb_ = [Buf("va%d" % i) for i in range(NB)]
                vones = Buf("vones")
                P.op("pool", lambda e: e.memset(vaug[:, :, :, 128:129], 1.0), writes=[vones])
                kpz = Buf("kpz")
                P.op("pool", lambda e: e.memset(kpc[64:128, :], 0.0), writes=[kpz])
                P.op("pool", lambda e: e.memset(qpt[0][64:128, :, :], 0.0), writes=[kpz])
                si = [0]
                for Tt in range(NT):
                    t0 = Tt * TW
                    P.dma("sp", lambda e, t0=t0: e.dma_start(out=kc[:, :, t0:t0 + TW], in_=dr["kn_d"][:, :, t0:t0 + TW].rearrange("h p t -> p h t")),
                          kcb[Tt], reads=[db["kn_d"][Tt]], writes=[kcb[Tt]])
                    P.dma("sp", lambda e, t0=t0: e.dma_start(out=kpc[0:64, t0:t0 + TW], in_=dr["kp_d"][:, t0:t0 + TW]),
                          kcb[Tt], reads=[db["kp_d"]], writes=[kcb[Tt]])
                    for blk in range(4):
                        i = Tt * 4 + blk
                        P.dma("sp", lambda e, i=i: e.dma_start(out=vaug[:, i, :, 0:128], in_=dr["v_d"][i].rearrange("p (h d) -> p h d", h=8)),
                              vb_[i], reads=[db["v_d"][i]], writes=[vb_[i]])
                    qb, qpb = qt[Tt % 2], qpt[Tt % 2]
                    load("sp", qb, qb[:], dr["qn_d"][:, :, t0:t0 + TW].rearrange("h p t -> p h t"), [db["qn_d"][Tt]])
                    load("sp", qpb, qpb[0:64, :, :], dr["qp_d"][:, :, t0:t0 + TW].rearrange("h p t -> p h t"), [db["qp_d"][Tt]])
                    nkb = 4 * Tt + 4
                    for h in range(8):
                        pend = None
                        for kb in range(nkb + 1):
                            if kb < nkb:
                                c0 = max(0, (kb - 4 * Tt)) * 128
                                sp_ = sps[si[0] % 3]
                                pt_ = pT[si[0] % 3]
                                si[0] += 1

                                def qk(e, sp_=sp_, kb=kb, c0=c0, h=h, qb=qb, qpb=qpb):
                                    e.matmul(sp_[:, c0:TW], lhsT=kc[:, h, kb * 128:(kb + 1) * 128], rhs=qb[:, h, c0:TW], start=True, stop=False)
                                    return e.matmul(sp_[:, c0:TW], lhsT=kpc[:, kb * 128:(kb + 1) * 128], rhs=qpb[:, h, c0:TW], start=False, stop=True)
                                P.op("pe", qk, reads=[kcb[kb // 4], qb.b, qpb.b, kpz], writes=[sp_.b])
                                P.op("act", lambda e, sp_=sp_, pt_=pt_, c0=c0: e.activation(out=pt_[:, c0:TW], in_=sp_[:, c0:TW], func=AF.Exp, scale=SM_SCALE),
                                     reads=[sp_.b], writes=[pt_.b])
                                if kb >= 4 * Tt:
                                    P.op("pool", lambda e, pt_=pt_, c0=c0: e.memset(pt_[64:128, c0:c0 + 64], 0.0), writes=[pt_.b])
                                cur = (kb, pt_, c0)
                            else:
                                cur = None
                            if pend is not None:
                                kbp, ptp, c0p = pend

                                def pv(e, kbp=kbp, ptp=ptp, c0p=c0p, h=h, Tt=Tt):
                                    ins = None
                                    for ql in range(c0p // 128, 4):
                                        qi = 4 * Tt + ql
                                        ins = e.matmul(ops_[ql][:, 0:129], lhsT=ptp[:, ql * 128:(ql + 1) * 128], rhs=vaug[:, kbp, h, :],
                                                       start=(kbp == 0), stop=(kbp == qi))
                                    return ins
                                P.op("pe", pv, reads=[ptp.b, vb_[kbp], vones], writes=[o.b for o in ops_[c0p // 128:]])
                                if kbp >= 4 * Tt:
                                    ql = kbp - 4 * Tt
                                    P.op("dve", lambda e, ql=ql: e.reciprocal(out=rs[:, ql:ql + 1], in_=ops_[ql][:, 128:129]),
                                         reads=[ops_[ql].b], writes=[rs.b])
                                    P.op("dve", lambda e, ql=ql, h=h: e.tensor_scalar(out=otok[:, ql, h * 128:(h + 1) * 128], in0=ops_[ql][:, 0:128],
                                                                                     scalar1=rs[:, ql:ql + 1], scalar2=None, op0=ALU.mult),
                                         reads=[ops_[ql].b, rs.b], writes=[otok.b])
                            pend = cur
                    ob = oT[Tt % 2]
                    for ql in range(4):
                        def tr(e, ql=ql):
                            for k in range(8):
                                ins = e.transpose(out=pto[:, k, :], in_=otok[:, ql, k * 128:(k + 1) * 128], identity=identb[:])
                            return ins
                        P.op("pe", tr, reads=[otok.b, identb.b], writes=[pto.b])
                        P.op("act", lambda e, ql=ql, ob=ob: e.copy(out=ob[:, :, ql * 128:(ql + 1) * 128], in_=pto[:]), reads=[pto.b], writes=[ob.b])
                    store("pool", ob, dr["o_d"][Tt], ob[:], [db["o_d"][Tt]])
                P.barrier()
                P.emit()
            if stop_after == 2:
                return nc

            with ExitStack() as SC:
                wing = sb(SC, "wing", [128, 8, 2048], BF16)
                wmo = sb(SC, "wmo", [128, 8, D], BF16)
                wout = sb(SC, "wout", [128, 8, D], BF16)
                wr = sb(SC, "wr", [128, 8, NE], F32)
                brB = sb(SC, "brB", [128, NE], F32)
                bgc = sb(SC, "bgc", [128, 16], F32)
                for k in range(8):
                    load("pool", wing, wing[:, k, :], dr["w_in"][k * 128:(k + 1) * 128, 2112:4160], max_dma_last_dim=4096)
                    load("pool", wmo, wmo[:, k, :], dr["w_mo"][k * 128:(k + 1) * 128, :])
                    load("pool", wout, wout[:, k, :], dr["w_out"][k * 128:(k + 1) * 128, :])
                load("sp", wr, wr[:], dr["w_r"].rearrange("(k p) n -> p k n", p=128))
                load("sp", brB, brB[:], dr["b_r"][0].partition_broadcast(128))
                load("sp", bgc, bgc[:], dr["b_gate"][:, :])
                hb = [sb(SC, "hb0", [128, 8, TW], BF16)] * 2
                ab = [sb(SC, "ab0", [128, 8, TW], BF16)] * 2
                ob = [sb(SC, "ob0", [128, 8, TW], BF16)] * 2
                modC = sb(SC, "modC", [128, 3 * D], F32)
                load("sp", modC, modC[:], dr["mod_d"][:, :], [db["mod_d"]])
                gA = sb(SC, "gA", [128, TW], F32)
                gB = sb(SC, "gB", [128, TW], F32)
                mt1 = sb(SC, "mt1", [128, TW], F32)
                mt2 = sb(SC, "mt2", [128, TW], F32)
                merged = sb(SC, "merged", [128, 8, TW], BF16)
                xt = [sb(SC, "xtc%d" % i, [128, D], F32) for i in range(2)]
                x1t = [sb(SC, "x1t%d" % i, [128, D], F32) for i in range(2)]
                tmpc = sb(SC, "tmpc", [128, D], F32)
                h2 = sb(SC, "h2", [128, D], F32)
                junk = sb(SC, "junkc", [128, D], BF16)
                ss2 = sb(SC, "ss2", [128, 1], F32)
                h2Tf = sb(SC, "h2Tf", [128, 8, 128], F32)
                h2bf = [sb(SC, "h2bf%d" % i, [128, D], BF16) for i in range(2)]
                lg = sb(SC, "lg", [128, NE], F32)
                m8 = sb(SC, "m8", [128, 8], F32)
                nm = sb(SC, "nm", [128, 1], F32)
                msk = sb(SC, "msk", [128, NE], F32)
                ex = sb(SC, "ex", [128, NE], F32)
                esum = sb(SC, "esum", [128, 1], F32)
                psA = ps(SC, "psA", [128, TW], F32)
                psB = ps(SC, "psB", [128, TW], F32)
                psM = ps(SC, "psM", [128, TW], F32)
                psX = [ps(SC, "psX%d" % i, [128, 512], F32) for i in range(2)]
                psT = [ps(SC, "psT%d" % i, [128, 4, 128], F32) for i in range(2)]
                psR = ps(SC, "psR", [128, 512], F32)

                def rstd2(src, dst, n):
                    P.op("dve", lambda e: e.tensor_scalar(out=dst[:], in0=src[:], scalar1=1.0 / n, scalar2=EPS, op0=ALU.mult, op1=ALU.add),
                         reads=[src.b], writes=[dst.b])
                    P.op("act", lambda e: e.activation(out=dst[:], in_=dst[:], func=AF.Sqrt), reads=[dst.b], writes=[dst.b])
                    P.op("dve", lambda e: e.reciprocal(out=dst[:], in_=dst[:]), reads=[dst.b], writes=[dst.b])

                for Tt in range(NT):
                    h_, a_, o_ = hb[Tt % 2], ab[Tt % 2], ob[Tt % 2]
                    load("sp", h_, h_[:], dr["h_d"][Tt], [db["h_d"][Tt]])
                    load("sp", a_, a_[:], dr["a_d"][Tt], [db["a_d"][Tt]])
                    load("sp", o_, o_[:], dr["o_d"][Tt], [db["o_d"][Tt]])
                    for mo in range(8):
                        def fa(e, mo=mo, h_=h_):
                            for k in range(8):
                                ins = e.matmul(psA[:], lhsT=wing[:, k, mo * 128:(mo + 1) * 128], rhs=h_[:, k, :], start=(k == 0), stop=(k == 7))
                            return ins

                        def fb(e, mo=mo, h_=h_):
                            for k in range(8):
                                ins = e.matmul(psB[:], lhsT=wing[:, k, 1024 + mo * 128:1024 + (mo + 1) * 128], rhs=h_[:, k, :], start=(k == 0), stop=(k == 7))
                            return ins

                        def fm(e, mo=mo, o_=o_):
                            for k in range(8):
                                ins = e.matmul(psM[:], lhsT=wmo[:, k, mo * 128:(mo + 1) * 128], rhs=o_[:, k, :], start=(k == 0), stop=(k == 7))
                            return ins
                        P.op("pe", fa, reads=[wing.b, h_.b], writes=[psA.b])
                        P.op("pe", fb, reads=[wing.b, h_.b], writes=[psB.b])
                        P.op("pe", fm, reads=[wmo.b, o_.b], writes=[psM.b])
                        P.op("act", lambda e, mo=mo: e.activation(out=gA[:], in_=psA[:], func=AF.Sigmoid, bias=bgc[:, mo:mo + 1]),
                             reads=[psA.b, bgc.b], writes=[gA.b])
                        P.op("act", lambda e, mo=mo: e.activation(out=gB[:], in_=psB[:], func=AF.Sigmoid, bias=bgc[:, 8 + mo:9 + mo]),
                             reads=[psB.b, bgc.b], writes=[gB.b])
                        P.op("pool", lambda e, mo=mo, a_=a_: e.tensor_tensor(out=mt1[:], in0=gA[:], in1=a_[:, mo, :], op=ALU.mult),
                             reads=[gA.b, a_.b], writes=[mt1.b])
                        P.op("dve", lambda e: e.tensor_tensor(out=mt2[:], in0=psM[:], in1=gB[:], op=ALU.mult),
                             reads=[psM.b, gB.b], writes=[mt2.b])
                        P.op("dve", lambda e, mo=mo: e.tensor_tensor(out=merged[:, mo, :], in0=mt1[:], in1=mt2[:], op=ALU.add),
                             reads=[mt1.b, mt2.b], writes=[merged.b])
                    def fx_emit(blk):
                        for j in range(2):
                            def fx(e, blk=blk, j=j):
                                for k in range(8):
                                    ins = e.matmul(psX[j][:], lhsT=merged[:, k, blk * 128:(blk + 1) * 128], rhs=wout[:, k, j * 512:(j + 1) * 512],
                                                   start=(k == 0), stop=(k == 7))
                                return ins
                            P.op("pe", fx, reads=[merged.b, wout.b], writes=[psX[j].b])
                    fx_emit(0)
                    for blk in range(4):
                        i = Tt * 4 + blk
                        xb_, x1b = xt[i % 2], x1t[i % 2]
                        load("sp", xb_, xb_[:], dr["x"][i * 128:(i + 1) * 128, :])
                        for j in range(2):
                            P.op("dve", lambda e, j=j: e.tensor_tensor(out=tmpc[:, j * 512:(j + 1) * 512], in0=psX[j][:], in1=modC[:, j * 512:(j + 1) * 512], op=ALU.mult),
                                 reads=[psX[j].b, modC.b], writes=[tmpc.b])
                        P.op("dve", lambda e, xb_=xb_, x1b=x1b: e.tensor_tensor(out=x1b[:], in0=tmpc[:], in1=xb_[:], op=ALU.add),
                             reads=[tmpc.b, xb_.b], writes=[x1b.b])
                        store("pool", x1b, dr["x1_d"][i], x1b[:], [db["x1_d"][i]])
                        P.op("act", lambda e, x1b=x1b: e.activation(out=junk[:], in_=x1b[:], func=AF.Square, accum_out=ss2[:]),
                             reads=[x1b.b], writes=[junk.b, ss2.b])
                        rstd2(ss2, ss2, D)
                        P.op("dve", lambda e, x1b=x1b: e.scalar_tensor_tensor(out=tmpc[:], in0=x1b[:], scalar=ss2[:, 0:1], in1=modC[:, 2 * D:3 * D], op0=ALU.mult, op1=ALU.mult),
                             reads=[x1b.b, ss2.b, modC.b], writes=[tmpc.b])
                        P.op("dve", lambda e: e.tensor_tensor(out=h2[:], in0=tmpc[:], in1=modC[:, D:2 * D], op=ALU.add),
                             reads=[tmpc.b, modC.b], writes=[h2.b])

                        if blk + 1 < 4:
                            fx_emit(blk + 1)
                        hbf = h2bf[i % 2]
                        P.op("act", lambda e, hbf=hbf: e.copy(out=hbf[:], in_=h2[:]), reads=[h2.b], writes=[hbf.b])
                        store("pool", hbf, dr["h2tok_d"][i * 128:(i + 1) * 128, :], hbf[:], [db["h2tok_d"][i]])

                        def trf(e):
                            for k in range(8):
                                ins = e.transpose(out=psT[k // 4][:, k % 4, :], in_=h2[:, k * 128:(k + 1) * 128], identity=identf[:])
                            return ins
                        P.op("pe", trf, reads=[h2.b, identf.b], writes=[psT[0].b, psT[1].b])
                        for hh in range(2):
                            P.op("act", lambda e, hh=hh: e.copy(out=h2Tf[:, hh * 4:(hh + 1) * 4, :], in_=psT[hh][:]), reads=[psT[hh].b], writes=[h2Tf.b])

                        def fr(e):
                            for k in range(8):
                                ins = e.matmul(psR[:, 0:NE], lhsT=h2Tf[:, k, :], rhs=wr[:, k, :], start=(k == 0), stop=(k == 7))
                            return ins
                        P.op("pe", fr, reads=[h2Tf.b, wr.b], writes=[psR.b])
                        P.op("dve", lambda e: e.tensor_tensor(out=lg[:], in0=psR[:, 0:NE], in1=brB[:], op=ALU.add), reads=[psR.b, brB.b], writes=[lg.b])
                        P.op("dve", lambda e: e.max(out=m8[:], in_=lg[:]), reads=[lg.b], writes=[m8.b])
                        P.op("dve", lambda e: e.tensor_scalar(out=nm[:], in0=m8[:, 0:1], scalar1=-1.0, scalar2=None, op0=ALU.mult), reads=[m8.b], writes=[nm.b])
                        P.op("dve", lambda e: e.tensor_scalar(out=msk[:], in0=lg[:], scalar1=m8[:, 3:4], scalar2=None, op0=ALU.is_ge), reads=[lg.b, m8.b], writes=[msk.b])
                        P.op("act", lambda e: e.activation(out=ex[:], in_=lg[:], func=AF.Exp, bias=nm[:, 0:1]), reads=[lg.b, nm.b], writes=[ex.b])
                        P.op("dve", lambda e: e.tensor_tensor(out=ex[:], in0=ex[:], in1=msk[:], op=ALU.mult), reads=[ex.b, msk.b], writes=[ex.b])
                        P.op("dve", lambda e: e.reduce_sum(out=esum[:], in_=ex[:], axis=AX.X), reads=[ex.b], writes=[esum.b])
                        P.op("dve", lambda e: e.reciprocal(out=esum[:], in_=esum[:]), reads=[esum.b], writes=[esum.b])
                        P.op("dve", lambda e, i=i: e.tensor_scalar(out=wdense[:, i, :], in0=ex[:], scalar1=esum[:, 0:1], scalar2=None, op0=ALU.mult),
                             reads=[ex.b, esum.b], writes=[wdense.b])
                if "wd_d" in dbg:
                    store("pool", wdense, dr["wd_d"][:, :], wdense[:].rearrange("p a b -> p (a b)"), [db["wd_d"]])
                P.barrier()
                P.emit()
        if stop_after == 3:
            return nc

        destI = sb(G, "destI", [128, 4, NB], I32)
        wkA = sb(G, "wkA", [128, 4, NB], F32)
        tbl = sb(G, "tbl", [128, 2, 4], I32)
        endI = sb(G, "endI", [128, NE], I32)
        with ExitStack() as SR:
            mskA = sb(SR, "mskA", [128, NB, NE], F32)
            mskAb = sb(SR, "mskAb", [128, NB * NE], BF16)
            utri = sb(SR, "utri", [128, 128], BF16)
            bcol = sb(SR, "bcol", [128, 2], F32)
            parB = sb(SR, "parB", [128, 2, NE], F32)
            posA = sb(SR, "posA", [128, NB, NE], F32)
            totA = sb(SR, "totA", [128, NB, NE], F32)
            cumB = sb(SR, "cumB", [128, NB, NE], F32)
            rk = [sb(SR, "rk%d" % i, [128, NB, NE], F32) for i in range(2)]
            selT = sb(SR, "selT", [128, NB, NE], F32)
            prodT = sb(SR, "prodT", [128, NB, NE], F32)
            destF = sb(SR, "destF", [128, 4, NB], F32)
            cnt = sb(SR, "cnt", [128, NE], F32)
            qv = sb(SR, "qv", [128, NE], F32)
            qi = sb(SR, "qi", [128, NE], I32)
            qf = sb(SR, "qf", [128, NE], F32)
            nbk = sb(SR, "nbk", [128, NE], F32)
            cs = [sb(SR, "cs%d" % i, [128, NE], F32) for i in range(2)]
            pst = sb(SR, "pst", [128, NE], F32)
            endS = sb(SR, "endS", [128, NE], F32)
            ele = sb(SR, "ele", [128, NE], F32)
            elp = sb(SR, "elp", [128, NE], F32)
            tblF = sb(SR, "tblF", [128, 2, 4], F32)
            nd = sb(SR, "nd", [128, 2], F32)
            psW = [ps(SR, "psW%d" % j, [128, 512], F32) for j in range(2)]
            psTt = [ps(SR, "psTt%d" % j, [128, 512], F32) for j in range(2)]
            load("sp", utri, utri[:], dr["utri"][:, :])
            load("sp", bcol, bcol[:], dr["bcol"][:, :])
            load("sp", parB, parB[:].rearrange("p a b -> p (a b)"), dr["par"][0].partition_broadcast(128))
            fl = lambda t_: t_[:].rearrange("p a b -> p (a b)")
            P.op("dve", lambda e: e.tensor_scalar(out=fl(mskA), in0=fl(wdense), scalar1=0.0, scalar2=None, op0=ALU.is_gt), reads=[wdense.b], writes=[mskA.b])
            P.op("dve", lambda e: e.tensor_copy(out=mskAb[:], in_=fl(mskA)), reads=[mskA.b], writes=[mskAb.b])
            for j in range(2):
                P.op("pe", lambda e, j=j: e.matmul(psW[j][:], lhsT=utri[:], rhs=mskAb[:, j * 512:(j + 1) * 512], start=True, stop=True),
                     reads=[utri.b, mskAb.b], writes=[psW[j].b])
                P.op("pe", lambda e, j=j: e.matmul(psTt[j][:], lhsT=onesb[:], rhs=mskAb[:, j * 512:(j + 1) * 512], start=True, stop=True),
                     reads=[onesb.b, mskAb.b], writes=[psTt[j].b])
                P.op("dve", lambda e, j=j: e.tensor_copy(out=fl(posA)[:, j * 512:(j + 1) * 512], in_=psW[j][:]), reads=[psW[j].b], writes=[posA.b])
                P.op("dve", lambda e, j=j: e.tensor_copy(out=fl(totA)[:, j * 512:(j + 1) * 512], in_=psTt[j][:]), reads=[psTt[j].b], writes=[totA.b])
            T0 = P.tick["pe"]
            P.op("dve", lambda e: e.memset(cumB[:, 0, :], 0.0), writes=[cumB.b])

            for i in range(1, NB):
                P.op("dve", lambda e, i=i: e.tensor_tensor(out=cumB[:, i, :], in0=cumB[:, i - 1, :], in1=totA[:, i - 1, :], op=ALU.add),
                     reads=[cumB.b, totA.b], writes=[cumB.b])
            P.op("dve", lambda e: e.tensor_tensor(out=cnt[:], in0=cumB[:, NB - 1, :], in1=totA[:, NB - 1, :], op=ALU.add), reads=[cumB.b, totA.b], writes=[cnt.b])
            P.op("dve", lambda e: e.tensor_scalar(out=qv[:], in0=cnt[:], scalar1=1.0 / 128, scalar2=None, op0=ALU.mult), reads=[cnt.b], writes=[qv.b])
            P.op("dve", lambda e: e.tensor_copy(out=qi[:], in_=qv[:]), reads=[qv.b], writes=[qi.b])
            P.op("dve", lambda e: e.tensor_copy(out=qf[:], in_=qi[:]), reads=[qi.b], writes=[qf.b])
            P.op("dve", lambda e: e.tensor_tensor(out=nbk[:], in0=qv[:], in1=qf[:], op=ALU.is_gt), reads=[qv.b, qf.b], writes=[nbk.b])
            P.op("dve", lambda e: e.tensor_tensor(out=nbk[:], in0=nbk[:], in1=qf[:], op=ALU.add), reads=[nbk.b, qf.b], writes=[nbk.b])
            P.op("dve", lambda e: e.tensor_copy(out=cs[0][:], in_=nbk[:]), reads=[nbk.b], writes=[cs[0].b])
            cur = 0
            for s_ in (1, 2, 4, 8, 16):
                a_, b_ = cs[cur], cs[1 - cur]
                P.op("dve", lambda e, a_=a_, b_=b_: e.tensor_copy(out=b_[:], in_=a_[:]), reads=[a_.b], writes=[b_.b])
                P.op("dve", lambda e, a_=a_, b_=b_, s_=s_: e.tensor_tensor(out=b_[:, s_:NE], in0=a_[:, s_:NE], in1=a_[:, 0:NE - s_], op=ALU.add),
                     reads=[a_.b], writes=[b_.b])
                cur = 1 - cur
            endb = cs[cur]
            P.op("dve", lambda e: e.tensor_tensor(out=pst[:], in0=endb[:], in1=nbk[:], op=ALU.subtract), reads=[endb.b, nbk.b], writes=[pst.b])
            P.op("dve", lambda e: e.tensor_scalar(out=pst[:], in0=pst[:], scalar1=128.0, scalar2=None, op0=ALU.mult), reads=[pst.b], writes=[pst.b])
            P.op("dve", lambda e: e.tensor_tensor(out=fl(posA), in0=fl(posA), in1=fl(cumB), op=ALU.add), reads=[posA.b, cumB.b], writes=[posA.b])

            def addp(e):
                for i in range(NB):
                    ins = e.tensor_tensor(out=posA[:, i, :], in0=posA[:, i, :], in1=pst[:], op=ALU.add)
                return ins
            P.op("dve", addp, reads=[posA.b, pst.b], writes=[posA.b])
            P.op("dve", lambda e: e.tensor_copy(out=fl(rk[0]), in_=fl(mskA)), reads=[mskA.b], writes=[rk[0].b])
            cur = 0
            for s_ in (1, 2, 4, 8, 16):
                a_, b_ = rk[cur], rk[1 - cur]
                P.op("dve", lambda e, a_=a_, b_=b_: e.tensor_copy(out=fl(b_), in_=fl(a_)), reads=[a_.b], writes=[b_.b])
                P.op("dve", lambda e, a_=a_, b_=b_, s_=s_: e.tensor_tensor(out=b_[:, :, s_:NE], in0=a_[:, :, s_:NE], in1=a_[:, :, 0:NE - s_], op=ALU.add),
                     reads=[a_.b], writes=[b_.b])
                cur = 1 - cur
            rnk = rk[cur]
            P.op("dve", lambda e: e.tensor_tensor(out=fl(rnk), in0=fl(rnk), in1=fl(mskA), op=ALU.subtract), reads=[rnk.b, mskA.b], writes=[rnk.b])
            for k in range(4):
                P.op("dve", lambda e, k=k: e.tensor_scalar(out=fl(selT), in0=fl(rnk), scalar1=float(k), scalar2=None, op0=ALU.is_equal), reads=[rnk.b], writes=[selT.b])
                P.op("dve", lambda e: e.tensor_tensor(out=fl(selT), in0=fl(selT), in1=fl(mskA), op=ALU.mult), reads=[selT.b, mskA.b], writes=[selT.b])
                P.op("dve", lambda e: e.tensor_tensor(out=fl(prodT), in0=fl(selT), in1=fl(posA), op=ALU.mult), reads=[selT.b, posA.b], writes=[prodT.b])
                P.op("dve", lambda e, k=k: e.reduce_sum(out=destF[:, k, :], in_=prodT[:], axis=AX.X), reads=[prodT.b], writes=[destF.b])
                P.op("dve", lambda e: e.tensor_tensor(out=fl(prodT), in0=fl(selT), in1=fl(wdense), op=ALU.mult), reads=[selT.b, wdense.b], writes=[prodT.b])
                P.op("dve", lambda e, k=k: e.reduce_sum(out=wkA[:, k, :], in_=prodT[:], axis=AX.X), reads=[prodT.b], writes=[wkA.b])
            P.op("dve", lambda e: e.tensor_copy(out=destI[:].rearrange("p a b -> p (a b)"), in_=destF[:].rearrange("p a b -> p (a b)")), reads=[destF.b], writes=[destI.b])
            P.op("dve", lambda e: e.memset(endS[:, 0:1], 0.0), writes=[endS.b])
            P.op("dve", lambda e: e.tensor_copy(out=endS[:, 1:NE], in_=endb[:, 0:NE - 1]), reads=[endb.b], writes=[endS.b])
            for j in range(2):
                P.op("dve", lambda e, j=j: e.tensor_scalar(out=ele[:], in0=endS[:], scalar1=bcol[:, j:j + 1], scalar2=None, op0=ALU.is_le),
                     reads=[endS.b, bcol.b], writes=[ele.b])
                for s_ in range(2):
                    P.op("dve", lambda e, s_=s_: e.tensor_tensor(out=elp[:], in0=ele[:], in1=parB[:, s_, :], op=ALU.mult), reads=[ele.b, parB.b], writes=[elp.b])
                    P.op("dve", lambda e, s_=s_: e.reduce_sum(out=nd[:, s_:s_ + 1], in_=elp[:], axis=AX.X), reads=[elp.b], writes=[nd.b])
                P.op("dve", lambda e, j=j: e.tensor_scalar(out=tblF[:, j, 0:2], in0=nd[:], scalar1=float(16 * 14), scalar2=None, op0=ALU.mult), reads=[nd.b], writes=[tblF.b])
                P.op("dve", lambda e, j=j: e.tensor_tensor(out=tblF[:, j, 2:3], in0=nd[:, 1:2], in1=nd[:, 0:1], op=ALU.subtract), reads=[nd.b], writes=[tblF.b])
                P.op("dve", lambda e, j=j: e.tensor_scalar(out=tblF[:, j, 2:3], in0=tblF[:, j, 2:3], scalar1=1.0, scalar2=None, op0=ALU.add), reads=[tblF.b], writes=[tblF.b])
                P.op("dve", lambda e, j=j: e.memset(tblF[:, j, 3:4], 0.0), writes=[tblF.b])
            P.op("dve", lambda e: e.tensor_copy(out=tbl[:].rearrange("p a b -> p (a b)"), in_=tblF[:].rearrange("p a b -> p (a b)")), reads=[tblF.b], writes=[tbl.b])
            P.op("dve", lambda e: e.tensor_scalar(out=qf[:], in0=endb[:], scalar1=5.0, scalar2=float(T0 + 4), op0=ALU.mult, op1=ALU.add), reads=[endb.b], writes=[qf.b])
            P.op("dve", lambda e: e.tensor_copy(out=endI[:], in_=qf[:]), reads=[qf.b], writes=[endI.b])
            if "tbl_d" in dbg:
                store("pool", tbl, dr["tbl_d"][:, 0:8], tbl[:].rearrange("p a b -> p (a b)"), [])
                store("pool", endI, dr["tbl_d"][:, 8:40], endI[:], [])
                store("pool", destI, dr["tbl_d"][:, 40:168], destI[:].rearrange("p a b -> p (a b)"), [])
            P.barrier()
            P.emit()
        if stop_after == 4:
            return nc

        NBLK = 160
        semw = [G.enter_context(nc.semaphore("semw%d" % i)) for i in range(2)]
        xs_gate = Buf("xs_gate")
        ysb_ = [Buf("ys%d" % b) for b in range(NBLK)]

        class Multi:
            def __init__(self, ins):
                self.ins = ins

            def then_inc(self, sem, n):
                for i_ in self.ins:
                    i_.then_inc(sem, n)

        with ExitStack() as SE:
            wslg = [sb(SE, "wslg%d" % i, [128, 9, 2 * D], BF16) for i in range(2)]
            wsld = [sb(SE, "wsld%d" % i, [128, 9, D], BF16) for i in range(2)]
            hrow = [sb(SE, "hrow%d" % i, [128, D], BF16) for i in range(4)]
            xrow = [sb(SE, "xrow%d" % i, [128, D], BF16) for i in range(2)]
            xT = [sb(SE, "xT%d" % i, [128, 8, 128], BF16) for i in range(2)]
            gt_ = sb(SE, "gt", [128, D], F32)
            sg_ = sb(SE, "sg", [128, D], F32)
            ut_ = sb(SE, "ut", [128, D], F32)
            act = [sb(SE, "act%d" % i, [128, D], BF16) for i in range(2)]
            actT = sb(SE, "actT", [128, 8, 128], BF16)
            ysb = [sb(SE, "ysb%d" % i, [128, D], F32) for i in range(2)]
            dmy = sb(SE, "dmy", [128, 8], F32)
            ptx = ps(SE, "ptx", [128, 8, 128], BF16)
            pta = ps(SE, "pta", [128, 8, 128], BF16)
            psGt = ps(SE, "psGt", [128, 2 * 512], F32)
            psUp = ps(SE, "psUp", [128, 2 * 512], F32)
            psY = ps(SE, "psY", [128, 2 * 512], F32)

            def wstream(g, lo=0, hi=NE):
                for ex_ in range(lo, hi):
                    s_ = ex_ % 2
                    if ex_ >= 2:
                        r = g.alloc_register("wr%d" % ex_)
                        g.reg_load(r, endI[0:1, ex_ - 2:ex_ - 1])
                        v = g.snap(r, donate=True)
                        g.wait_ge(P.sem["pe"], v)
                        g.free_register(r)
                    for k in range(8):
                        g.dma_start(out=wslg[s_][:, k, :], in_=dr["w_gu"][ex_, k * 128:(k + 1) * 128, :], max_dma_last_dim=8192).then_inc(semw[s_], 16)
                    g.dma_start(out=wslg[s_][0:1, 8, :], in_=dr["b_gu_r"][ex_:ex_ + 1, :], max_dma_last_dim=8192).then_inc(semw[s_], 16)
                    for kk in range(4):
                        g.dma_start(out=wsld[s_][:, 2 * kk:2 * kk + 2, :], in_=dr["w_dn"][ex_].rearrange("(k p) n -> p k n", p=128)[:, 2 * kk:2 * kk + 2, :]).then_inc(semw[s_], 16)
                    g.dma_start(out=wsld[s_][0:1, 8, :], in_=dr["b_dn"][ex_:ex_ + 1, :]).then_inc(semw[s_], 16)

            P.ops["pool"].append(lambda g: wstream(g, 0, 2))
            scs = [[Buf("scs%d_%d" % (a_, k_)) for k_ in range(4)] for a_ in range(4)]
            for i in range(NB):
                hr = hrow[i % 4]
                load("sp", hr, hr[:], dr["h2tok_d"][i * 128:(i + 1) * 128, :], [db["h2tok_d"][i]])
                for k in range(4):
                    P.dma("pool", lambda e, hr=hr, i=i, k=k: e.indirect_dma_start(
                        out=dr["xs_d"][:, :], out_offset=bass.IndirectOffsetOnAxis(ap=destI[:, k, i:i + 1], axis=0),
                        in_=hr[:], in_offset=None), scs[i % 4][k], reads=[hr.b, destI.b, xs_gate], writes=[])
            P.op("pool", lambda e: e.memset(dmy[:], 0.0), writes=[xs_gate])
            P.ops["pool"].append(lambda g: wstream(g, 2, NE))

            slotreg = {}

            def emit_tx(b):
                xr, xt_ = xrow[b % 2], xT[b % 2]
                load("sp", xr, xr[:], dr["xs_d"][b * 128:(b + 1) * 128, :], [xs_gate])

                def tr(e, xr=xr):
                    for k in range(8):
                        ins = e.transpose(out=ptx[:, k, :], in_=xr[:, k * 128:(k + 1) * 128], identity=identb[:])
                    return ins
                P.op("pe", tr, reads=[xr.b, identb.b], writes=[ptx.b])
                P.op("act", lambda e, xt_=xt_: e.copy(out=xt_[:], in_=ptx[:]), reads=[ptx.b], writes=[xt_.b])

            def emit_gu(b):
                xt_ = xT[b % 2]

                def gu_part(e, b=b, xt_=xt_, part=0):
                    p_, j_ = b % 128, b // 128
                    if part == 0:
                        regs = []
                        for c in range(3):
                            r = e.alloc_register("br%d_%d" % (b, c))
                            e.reg_load(r, tbl[p_:p_ + 1, j_, c:c + 1])
                            regs.append(r)
                        vals = [e.snap(r, donate=True) for r in regs]
                        e.wait_ge(semw[0], vals[0])
                        e.wait_ge(semw[1], vals[1])
                        e.free_register(regs[0])
                        e.free_register(regs[1])
                        slotreg[b] = (regs[2], vals[2])
                    sl = slotreg[b][1]
                    lasts = []

                    def body(s_):
                        pt_ = psGt if part == 0 else psUp
                        for n2 in range(2):
                            n = part * 2 + n2
                            o_ = pt_[:, n2 * 512:(n2 + 1) * 512]
                            for k in range(8):
                                e.matmul(o_, lhsT=xt_[:, k, :], rhs=wslg[s_][:, k, n * 512:(n + 1) * 512], start=(k == 0), stop=False)
                            ins = e.matmul(o_, lhsT=onesb[0:1, :], rhs=wslg[s_][0:1, 8, n * 512:(n + 1) * 512], start=False, stop=True)
                        return ins
                    with e.If(sl == 0):
                        lasts.append(body(0))
                    with e.Else():
                        lasts.append(body(1))
                    return Multi(lasts)
                P.op("pe", lambda e: gu_part(e, part=0), reads=[xt_.b, onesb.b], writes=[psGt.b])
                P.op("pe", lambda e: gu_part(e, part=1), reads=[xt_.b, onesb.b], writes=[psUp.b])

            def emit_ew_a(b):
                P.op("dve", lambda e: e.tensor_scalar(out=gt_[:], in0=psGt[:], scalar1=7.0, scalar2=None, op0=ALU.min), reads=[psGt.b], writes=[gt_.b])
                P.op("act", lambda e: e.activation(out=sg_[:], in_=gt_[:], func=AF.Sigmoid, scale=1.702), reads=[gt_.b], writes=[sg_.b])
                P.op("dve", lambda e: e.tensor_scalar(out=ut_[:], in0=psUp[:], scalar1=7.0, scalar2=-7.0, op0=ALU.min, op1=ALU.max), reads=[psUp.b], writes=[ut_.b])

            def emit_ew_b(b):
                ab_ = act[b % 2]
                P.op("dve", lambda e: e.tensor_tensor(out=sg_[:], in0=gt_[:], in1=sg_[:], op=ALU.mult), reads=[gt_.b, sg_.b], writes=[sg_.b])
                P.op("dve", lambda e, ab_=ab_: e.scalar_tensor_tensor(out=ab_[:], in0=ut_[:], scalar=1.0, in1=sg_[:], op0=ALU.add, op1=ALU.mult),
                     reads=[ut_.b, sg_.b], writes=[ab_.b])

            def emit_ta(b):
                ab_ = act[b % 2]

                def tr(e, ab_=ab_):
                    for k in range(8):
                        ins = e.transpose(out=pta[:, k, :], in_=ab_[:, k * 128:(k + 1) * 128], identity=identb[:])
                    return ins
                P.op("pe", tr, reads=[ab_.b, identb.b], writes=[pta.b])
                P.op("act", lambda e: e.copy(out=actT[:], in_=pta[:]), reads=[pta.b], writes=[actT.b])

            def emit_down(b):
                yb = ysb[b % 2]

                def dn(e, b=b):
                    reg, sl = slotreg[b]
                    lasts = []

                    def body(s_):
                        for j in range(2):
                            o_ = psY[:, j * 512:(j + 1) * 512]
                            for k in range(8):
                                e.matmul(o_, lhsT=actT[:, k, :], rhs=wsld[s_][:, k, j * 512:(j + 1) * 512], start=(k == 0), stop=False)
                            ins = e.matmul(o_, lhsT=onesb[0:1, :], rhs=wsld[s_][0:1, 8, j * 512:(j + 1) * 512], start=False, stop=True)
                        return ins
                    with e.If(sl == 0):
                        lasts.append(body(0))
                    with e.Else():
                        lasts.append(body(1))
                    e.free_register(reg)
                    return Multi(lasts)
                P.op("pe", dn, reads=[actT.b, onesb.b], writes=[psY.b])
                P.op("act", lambda e, yb=yb: e.copy(out=yb[:], in_=psY[:]), reads=[psY.b], writes=[yb.b])
                P.dma("act", lambda e, yb=yb, b=b: e.dma_start(out=dr["ys_d"][b * 128:(b + 1) * 128, :], in_=yb[:]), yb.b, reads=[yb.b], writes=[ysb_[b]])

            assert P.tick["pe"] == T0
            dummy = lambda: P.op("pe", lambda e: e.transpose(out=ptx[:, 0, :], in_=identb[:], identity=identb[:]), reads=[identb.b], writes=[ptx.b])
            emit_tx(0)
            emit_tx(1)
            emit_gu(0)
            emit_ew_a(0)
            emit_ew_b(0)
            for b in range(NBLK):
                if b + 1 < NBLK:
                    emit_gu(b + 1)
                    emit_ew_a(b + 1)
                else:
                    dummy()
                    dummy()
                emit_ta(b)
                if b + 2 < NBLK:
                    emit_tx(b + 2)
                else:
                    dummy()
                emit_down(b)
                assert P.tick["pe"] == T0 + 4 + 5 * (b + 1)
                if b + 1 < NBLK:
                    emit_ew_b(b + 1)
            P.barrier()
            P.emit()
        if stop_after == 5:
            return nc

        with ExitStack() as SF:
            yk = [sb(SF, "yk%d" % i, [128, D], F32) for i in range(8)]
            accf = [sb(SF, "accf%d" % i, [128, D], F32) for i in range(2)]
            x1b = [sb(SF, "x1b%d" % i, [128, D], F32) for i in range(2)]
            tmpd = sb(SF, "tmpd", [128, D], F32)
            ob_ = [sb(SF, "obf%d" % i, [128, D], F32) for i in range(2)]
            junk = sb(SF, "junkf", [128, D], BF16)
            ss3 = sb(SF, "ss3", [128, 1], F32)
            def gathers(i):
                for k in range(4):
                    y_ = yk[(i * 4 + k) % 8]
                    P.dma("pool", lambda e, y_=y_, i=i, k=k: e.indirect_dma_start(
                        out=y_[:], out_offset=None, in_=dr["ys_d"][:, :],
                        in_offset=bass.IndirectOffsetOnAxis(ap=destI[:, k, i:i + 1], axis=0)), y_.b, reads=[destI.b], writes=[y_.b])
            gathers(0)
            tmps = [tmpd, sb(SF, "tmpe", [128, D], F32)]
            ss3s = [ss3, sb(SF, "ss3b", [128, 1], F32)]

            def fin_a(i):
                xb_, ac, tp, s3 = x1b[i % 2], accf[i % 2], tmps[i % 2], ss3s[i % 2]
                P.op("dve", lambda e: e.tensor_tensor(out=tp[:], in0=ac[:], in1=lateB[:, 0, :], op=ALU.mult), reads=[ac.b, lateB.b], writes=[tp.b])
                P.op("dve", lambda e: e.tensor_tensor(out=xb_[:], in0=tp[:], in1=xb_[:], op=ALU.add), reads=[tp.b, xb_.b], writes=[xb_.b])
                P.op("act", lambda e: e.activation(out=junk[:], in_=xb_[:], func=AF.Square, accum_out=s3[:]), reads=[xb_.b], writes=[junk.b, s3.b])

            def fin_b(i):
                xb_, o_, tp, s3 = x1b[i % 2], ob_[i % 2], tmps[i % 2], ss3s[i % 2]
                P.op("dve", lambda e: e.tensor_scalar(out=s3[:], in0=s3[:], scalar1=1.0 / D, scalar2=EPS, op0=ALU.mult, op1=ALU.add), reads=[s3.b], writes=[s3.b])
                P.op("act", lambda e: e.activation(out=s3[:], in_=s3[:], func=AF.Sqrt), reads=[s3.b], writes=[s3.b])
                P.op("dve", lambda e: e.reciprocal(out=s3[:], in_=s3[:]), reads=[s3.b], writes=[s3.b])
                P.op("dve", lambda e: e.scalar_tensor_tensor(out=tp[:], in0=xb_[:], scalar=s3[:, 0:1], in1=lateB[:, 1, :], op0=ALU.mult, op1=ALU.mult),
                     reads=[xb_.b, s3.b, lateB.b], writes=[tp.b])
                P.op("dve", lambda e: e.tensor_tensor(out=o_[:], in0=tp[:], in1=lateB[:, 2, :], op=ALU.add), reads=[tp.b, lateB.b], writes=[o_.b])
                store("sp", o_, dr["out"][i * 128:(i + 1) * 128, :], o_[:], [db["out"][i]])

            for i in range(NB):
                xb_, ac = x1b[i % 2], accf[i % 2]
                load("sp", xb_, xb_[:], dr["x1_d"][i], [db["x1_d"][i]])
                if i + 1 < NB:
                    gathers(i + 1)
                for k in range(4):
                    y_ = yk[(i * 4 + k) % 8]
                    if k == 0:
                        P.op("act", lambda e, y_=y_, ac=ac, i=i, k=k: e.activation(out=ac[:], in_=y_[:], func=AF.Identity, scale=wkA[:, k, i:i + 1]),
                             reads=[y_.b, wkA.b], writes=[ac.b])
                    else:
                        P.op("dve", lambda e, y_=y_, ac=ac, i=i, k=k: e.scalar_tensor_tensor(out=ac[:], in0=y_[:], scalar=wkA[:, k, i:i + 1], in1=ac[:], op0=ALU.mult, op1=ALU.add),
                             reads=[y_.b, wkA.b, ac.b], writes=[ac.b])
                fin_a(i)
                if i >= 1:
                    fin_b(i - 1)
            fin_b(NB - 1)
            P.barrier()
            P.emit()
    return nc


def _consts():
    rm = np.zeros((64, 64), np.float32)
    for m in range(32):
        rm[m + 32, m] = -1.0
        rm[m, m + 32] = 1.0
    invf = (1.0 / (10000.0 ** (np.arange(0, 64, 2, dtype=np.float32) / 64))).astype(np.float32)
    invf = np.tile(invf, 4).reshape(128, 1).astype(np.float32)
    t = np.arange(TW)
    rc0 = np.stack([1.0 / np.minimum(t[:16] + 1, w) for w in (2, 4, 8, 16)]).astype(np.float32).reshape(1, 64)
    return {
        "identb": np.eye(128, dtype=np.float32).astype(ml_dtypes.bfloat16),
        "identf": np.eye(128, dtype=np.float32),
        "rmat": rm.astype(ml_dtypes.bfloat16),
        "invf": invf, "rc0": rc0,
        "utri": np.triu(np.ones((128, 128), np.float32), 1).astype(ml_dtypes.bfloat16),
        "bcol": np.stack([np.arange(128), np.arange(128) + 128], 1).astype(np.float32),
        "par": np.concatenate([(np.arange(NE) % 2 == 0), (np.arange(NE) % 2 == 1)]).astype(np.float32).reshape(1, 2 * NE),
    }


def _colT(v, n):
    return np.ascontiguousarray(np.asarray(v, np.float32).reshape(n, 128).T)


def prep_shared(inp):
    f = lambda a: np.ascontiguousarray(np.asarray(a, np.float32))
    qperm = np.concatenate([np.arange(h * 192, h * 192 + 128) for h in range(8)] + [np.arange(h * 192 + 128, h * 192 + 192) for h in range(8)])
    kvperm = np.concatenate([np.arange(h * 256, h * 256 + 128) for h in range(8)] + [np.arange(h * 256 + 128, h * 256 + 256) for h in range(8)])
    guperm = np.concatenate([np.arange(0, 2 * D, 2), np.arange(1, 2 * D, 2)])
    b_gu = np.asarray(inp["b_gu"], np.float32)[0][:, guperm]
    b_gu_c = np.ascontiguousarray(b_gu.reshape(NE, 16, 128).transpose(2, 0, 1).reshape(128, NE * 16))
    sh = {
        "w_mod": f(inp["w_mod"][0]), "b_mod": f(inp["b_mod"][0]).reshape(1, -1), "g_mix": _colT(inp["g_mix"][0], 8),
        "w_in": f(inp["w_in"][0]), "b_gate": _colT(inp["b_gate"][0], 16),
        "w_grp": f(inp["w_pool_grp"][0]), "pscale": _colT(inp["pool_scale"][0], 8), "w_proj": f(inp["w_pool_out"][0]),
        "g_q": _colT(inp["g_q_a"][0], 6), "w_q": f(np.asarray(inp["w_q_b"][0])[:, qperm]),
        "g_kv": _colT(inp["g_kv_a"][0], 2), "w_kv": f(np.asarray(inp["w_kv_b"][0])[:, kvperm]),
        "w_mo": f(inp["w_mla_out"][0]), "w_out": f(inp["w_out"][0]), "g_ffn": f(inp["g_ffn"][0]).reshape(1, -1),
        "w_r": f(inp["w_router"][0]), "b_r": f(inp["b_router"][0]).reshape(1, -1),
        "w_gu": f(np.asarray(inp["w_gu"][0])[:, :, guperm]), "b_gu": b_gu_c, "b_gu_r": f(b_gu),
        "w_dn": f(inp["w_down"][0]), "b_dn": f(inp["b_down"][0]),
        "g_fin": f(inp["g_final"]).reshape(1, -1), "w_fmod": f(inp["w_fmod"]), "b_fmod": f(inp["b_fmod"]).reshape(1, -1),
    }
    sh.update(_consts())
    return sh


def core_inputs(inp, sh, b):
    m = dict(sh)
    m["x"] = np.ascontiguousarray(np.asarray(inp["x"][b], np.float32))
    m["cT"] = _colT(inp["c"][b], 8)
    m["pos"] = np.ascontiguousarray(np.asarray(inp["positions"][b], np.int32).reshape(1, S))
    return m


def kernel(**inputs):
    sh = prep_shared(inputs)
    nc = build()
    in_maps = [core_inputs(inputs, sh, b) for b in range(8)]
    res = run_bass_kernel_spmd(nc, in_maps, core_ids=list(range(8)))
    return np.stack([np.asarray(res.results[b]["out"], np.float32) for b in range(8)], axis=0)
```

```python
import numpy as np
from contextlib import ExitStack
import ml_dtypes
import concourse.bass as bass
import concourse.mybir as mybir
from concourse.bass_utils import run_bass_kernel_spmd

F32 = mybir.dt.float32
BF16 = mybir.dt.bfloat16
I32 = mybir.dt.int32
AF = mybir.ActivationFunctionType
ALU = mybir.AluOpType
AX = mybir.AxisListType

S = 4096
D = 1024
NT = 8
TW = 512
NB = 32
NE = 32
EPS = 1e-6
SM_SCALE = 192 ** -0.5
CUT = 99
SKIP = set()
TWO_PI = 6.283185307179586
PI = 3.141592653589793


class Buf:
    __slots__ = ("name", "w", "r", "dsem", "dtot")

    def __init__(self, name):
        self.name = name
        self.w = None
        self.r = []
        self.dsem = None
        self.dtot = 0


class Prog:
    ENG = ("pe", "act", "dve", "pool", "sp")

    def __init__(self, nc, stack):
        self.nc = nc
        self.stack = stack
        self.ops = {e: [] for e in self.ENG}
        self.sem = {e: stack.enter_context(nc.semaphore("s_" + e)) for e in self.ENG}
        self.tick = {e: 0 for e in self.ENG}
        self.seen = {e: {} for e in self.ENG}
        self.semobj = {}
        self.dbufs = []
        self.free = []
        for e in self.ENG:
            self.semobj[("eng", e)] = self.sem[e]
        self.nd = 0

    def _need(self, eng, toks):
        best = {}
        for t in toks:
            if t is None:
                continue
            k, v = t
            if k == ("eng", eng) and eng == "pe":
                continue
            if best.get(k, 0) < v:
                best[k] = v
        waits = []
        for k, v in best.items():
            if self.seen[eng].get(k, 0) >= v:
                continue
            self.seen[eng][k] = v
            waits.append((self.semobj[k], v))
        return waits

    @staticmethod
    def _deps(reads, writes):
        toks = []
        for b in reads:
            toks.append(b.w)
        for b in writes:
            toks.append(b.w)
            toks.extend(b.r)
        return toks

    @staticmethod
    def _mark(tok, reads, writes):
        for b in reads:
            b.r.append(tok)
        for b in writes:
            b.w = tok
            b.r = []

    def op(self, eng, fn, reads=(), writes=()):
        waits = self._need(eng, self._deps(reads, writes))
        self.tick[eng] += 1
        tok = (("eng", eng), self.tick[eng])
        sem = self.sem[eng]

        def run(e, fn=fn, waits=waits, sem=sem):
            for s_, v in waits:
                e.wait_ge(s_, v)
            fn(e).then_inc(sem, 1)
        self.ops[eng].append(run)
        self._mark(tok, reads, writes)
        return tok

    def dma(self, q, fn, sb, reads=(), writes=()):
        if sb.dsem is None:
            if self.free:
                sb.dsem = self.free.pop()
            else:
                self.nd += 1
                h = self.stack.enter_context(self.nc.semaphore("d%d" % self.nd))
                sb.dsem = [h, 0, self.nd]
                self.semobj[("dma", self.nd)] = h
            self.dbufs.append(sb)
        ds = sb.dsem
        key = ("dma", ds[2])
        toks = self._deps(reads, writes)
        if ds[1] > 0:
            toks.append((key, ds[1]))
        waits = self._need(q, toks)
        ds[1] += 16
        tok = (key, ds[1])
        sem = ds[0]

        def run(e, fn=fn, waits=waits, sem=sem):
            for s_, v in waits:
                e.wait_ge(s_, v)
            fn(e).then_inc(sem, 16)
        self.ops[q].append(run)
        self._mark(tok, reads, writes)
        return tok

    def barrier(self):
        toks = [(("eng", e), self.tick[e]) for e in self.ENG if self.tick[e] > 0]
        toks += [(("dma", b.dsem[2]), b.dsem[1]) for b in self.dbufs]
        for e in self.ENG:
            waits = self._need(e, toks)

            def run(en, waits=waits):
                for s_, v in waits:
                    en.wait_ge(s_, v)
            self.ops[e].append(run)
        for b in self.dbufs:
            self.free.append(b.dsem)
            b.dsem = None
        self.dbufs = []

    def emit(self):
        nc = self.nc
        ops = self.ops
        with nc.Block() as block:
            @block.tensor
            def _(e):
                for f in ops["pe"]:
                    f(e)

            @block.scalar
            def _(e):
                for f in ops["act"]:
                    f(e)

            @block.vector
            def _(e):
                for f in ops["dve"]:
                    f(e)

            @block.gpsimd
            def _(e):
                for f in ops["pool"]:
                    f(e)

            @block.sync
            def _(e):
                for f in ops["sp"]:
                    f(e)
        self.ops = {e: [] for e in self.ENG}


class T:
    def __init__(self, t, name):
        self.t = t
        self.b = Buf(name)

    def __getitem__(self, idx):
        return self.t[idx]


IN_SPECS = [
    ("x", [S, D], F32), ("cT", [128, 8], F32), ("pos", [1, S], I32),
    ("w_mod", [D, 6 * D], F32), ("b_mod", [1, 6 * D], F32), ("g_mix", [128, 8], F32),
    ("w_in", [D, 4160], F32), ("b_gate", [128, 16], F32),
    ("w_grp", [4, 256, 256], F32), ("pscale", [128, 8], F32), ("w_proj", [D, D], F32),
    ("g_q", [128, 6], F32), ("w_q", [768, 1536], F32), ("g_kv", [128, 2], F32), ("w_kv", [256, 2048], F32),
    ("w_mo", [D, D], F32), ("w_out", [D, D], F32), ("g_ffn", [1, D], F32),
    ("w_r", [D, NE], F32), ("b_r", [1, NE], F32),
    ("w_gu", [NE, D, 2 * D], F32), ("b_gu", [128, NE * 16], F32), ("w_dn", [NE, D, D], F32), ("b_dn", [NE, D], F32),
    ("g_fin", [1, D], F32), ("w_fmod", [D, 2 * D], F32), ("b_fmod", [1, 2 * D], F32),
    ("identb", [128, 128], BF16), ("identf", [128, 128], F32), ("rmat", [64, 64], BF16),
    ("invf", [128, 1], F32), ("rc0", [1, 64], F32),
    ("utri", [128, 128], BF16), ("bcol", [128, 2], F32), ("par", [1, 2 * NE], F32), ("b_gu_r", [NE, 2 * D], F32),
]

SCRATCH = [
    ("cos_d", [64, S], F32), ("sin_d", [64, S], F32),
    ("h_d", [NT, 128, 8, TW], BF16), ("a_d", [NT, 128, 8, TW], BF16),
    ("qn_d", [8, 128, S], BF16), ("qp_d", [8, 64, S], BF16),
    ("kn_d", [8, 128, S], BF16), ("kp_d", [64, S], BF16), ("v_d", [NB, 128, D], BF16),
    ("o_d", [NT, 128, 8, TW], BF16), ("x1_d", [NB, 128, D], F32), ("h2_d", [NT, 128, 8, TW], BF16),
    ("wd_d", [128, NB * NE], F32), ("mod_d", [128, 3 * D], F32),
    ("h2tok_d", [S, D], BF16), ("xs_d", [160 * 128, D], BF16), ("ys_d", [160 * 128, D], F32), ("tbl_d", [128, 168], I32),
]


def build(dbg=(), stop_after=None):
    nc = bass.Bass("TRN2", target_bir_lowering=False)
    dr = {}
    for n, shp, dt in IN_SPECS:
        dr[n] = nc.dram_tensor(n, shp, dt, kind="ExternalInput").ap()
    for n, shp, dt in SCRATCH:
        kind = "ExternalOutput" if n in dbg else "Internal"
        dr[n] = nc.dram_tensor(n, shp, dt, kind=kind).ap()
    dr["out"] = nc.dram_tensor("out", [S, D], F32, kind="ExternalOutput").ap()
    db = {}
    for n in ("cos_d", "sin_d", "kp_d", "wd_d", "mod_d"):
        db[n] = Buf(n)
    for n in ("h_d", "a_d", "o_d", "h2_d"):
        db[n] = [Buf(n + str(i)) for i in range(NT)]
    for n in ("qn_d", "qp_d", "kn_d"):
        db[n] = [Buf(n + str(i)) for i in range(NT)]
    db["h2tok_d"] = [Buf("h2tok" + str(i)) for i in range(NB)]
    for n in ("v_d", "x1_d"):
        db[n] = [Buf(n + str(i)) for i in range(NB)]
    db["out"] = [Buf("out" + str(i)) for i in range(NB)]

    with ExitStack() as G:
        P = Prog(nc, G)

        def sb(st, name, shape, dt):
            return T(st.enter_context(nc.sbuf_tensor("s_" + name, shape, dt)), name)

        def ps(st, name, shape, dt):
            return T(st.enter_context(nc.psum_tensor("p_" + name, shape, dt)), name)

        def load(q, dst, dst_ap, src_ap, srcbufs=(), **kw):
            P.dma(q, lambda e: e.dma_start(out=dst_ap, in_=src_ap, **kw), dst.b, reads=list(srcbufs), writes=[dst.b])

        def store(q, src, dst_ap, src_ap, dstbufs=()):
            P.dma(q, lambda e: e.dma_start(out=dst_ap, in_=src_ap), src.b, reads=[src.b], writes=list(dstbufs))

        identb = sb(G, "identb", [128, 128], BF16)
        identf = sb(G, "identf", [128, 128], F32)
        onesf = sb(G, "onesf", [128, 128], F32)
        onesb = sb(G, "onesb", [128, 128], BF16)
        lateB = sb(G, "lateB", [128, 3, D], F32)
        wdense = sb(G, "wdense", [128, NB, NE], F32)
        bguc = sb(G, "bguc", [128, NE * 16], F32)
        load("sp", identb, identb[:], dr["identb"][:, :])
        load("sp", identf, identf[:], dr["identf"][:, :])
        load("sp", bguc, bguc[:], dr["b_gu"][:, :])
        P.op("pool", lambda e: e.memset(onesf[:], 1.0), writes=[onesf.b])
        P.op("pool", lambda e: e.memset(onesb[:], 1.0), writes=[onesb.b])

        with ExitStack() as S1:
            A1c = sb(S1, "A1c", [128, 8], F32)
            sh1c = sb(S1, "sh1c", [128, 8], F32)

            with ExitStack() as SA:
                wina = sb(SA, "wina", [128, 8, 2112], BF16)
                wgrp = sb(SA, "wgrp", [128, 4, 2, 256], BF16)
                wproj = sb(SA, "wproj", [128, 8, D], BF16)
                wq = sb(SA, "wq", [128, 6, 1536], BF16)
                wkv = sb(SA, "wkv", [128, 2, 2048], BF16)
                for k in range(8):
                    load("pool", wina, wina[:, k, :], dr["w_in"][k * 128:(k + 1) * 128, 0:2112], max_dma_last_dim=4096)
                load("pool", wgrp, wgrp[:], dr["w_grp"].rearrange("g (ki p) d -> p g ki d", p=128))
                for k in range(8):
                    load("pool", wproj, wproj[:, k, :], dr["w_proj"][k * 128:(k + 1) * 128, :])
                for k in range(6):
                    load("pool", wq, wq[:, k, :], dr["w_q"][k * 128:(k + 1) * 128, :], max_dma_last_dim=4096)
                for k in range(2):
                    load("pool", wkv, wkv[:, k, :], dr["w_kv"][k * 128:(k + 1) * 128, :], max_dma_last_dim=4096)
                with ExitStack() as S0:
                    modB = sb(S0, "modB", [128, 6 * D], F32)
                    fmodB = sb(S0, "fmodB", [128, 2 * D], F32)
                    csb = sb(S0, "csb", [128, 8], F32)
                    cact = sb(S0, "cact", [128, 8], F32)
                    crep = sb(S0, "crep", [128, 8, 128], F32)
                    gmc = sb(S0, "gmc", [128, 8], F32)
                    gffB = sb(S0, "gffB", [128, D], F32)
                    gfinB = sb(S0, "gfinB", [128, D], F32)
                    wt = [sb(S0, "wt%d" % i, [128, 8, 512], F32) for i in range(2)]
                    pm = [ps(S0, "pm%d" % i, [128, 512], F32) for i in range(2)]
                    dtmp = sb(S0, "dtmp", [128, 128], F32)
                    dcol = sb(S0, "dcol", [128, 16], F32)
                    load("sp", csb, csb[:], dr["cT"][:, :])
                    load("sp", gmc, gmc[:], dr["g_mix"][:, :])
                    load("sp", modB, modB[:], dr["b_mod"][0].partition_broadcast(128))
                    load("sp", fmodB, fmodB[:], dr["b_fmod"][0].partition_broadcast(128))
                    load("sp", gffB, gffB[:], dr["g_ffn"][0].partition_broadcast(128))
                    load("sp", gfinB, gfinB[:], dr["g_fin"][0].partition_broadcast(128))
                    P.op("act", lambda e: e.activation(out=cact[:], in_=csb[:], func=AF.Silu), reads=[csb.b], writes=[cact.b])

                    def mk_crep(e):
                        for k in range(8):
                            i = e.tensor_scalar(out=crep[:, k, :], in0=onesf[:], scalar1=cact[:, k:k + 1], scalar2=None, op0=ALU.mult)
                        return i
                    P.op("dve", mk_crep, reads=[onesf.b, cact.b], writes=[crep.b])
                    for ci in range(16):
                        w_src = dr["w_mod"] if ci < 12 else dr["w_fmod"]
                        c0 = (ci if ci < 12 else ci - 12) * 512
                        tgt = modB if ci < 12 else fmodB
                        wb = wt[ci % 2]
                        pb = pm[ci % 2]
                        load("sp", wb, wb[:], w_src.rearrange("(k p) n -> p k n", p=128)[:, :, c0:c0 + 512])

                        def mm(e, wb=wb, pb=pb):
                            for k in range(8):
                                i = e.matmul(pb[:], lhsT=crep[:, k, :], rhs=wb[:, k, :], start=(k == 0), stop=(k == 7))
                            return i
                        P.op("pe", mm, reads=[crep.b, wb.b], writes=[pb.b])
                        P.op("dve", lambda e, pb=pb, tgt=tgt, c0=c0: e.tensor_tensor(
                            out=tgt[:, c0:c0 + 512], in0=pb[:], in1=tgt[:, c0:c0 + 512], op=ALU.add),
                            reads=[pb.b, tgt.b], writes=[tgt.b])
                    for j in range(16):
                        c0 = j * 128
                        P.op("dve", lambda e, c0=c0: e.tensor_tensor(out=dtmp[:], in0=modB[:, c0:c0 + 128], in1=identf[:], op=ALU.mult),
                             reads=[modB.b, identf.b], writes=[dtmp.b])
                        P.op("dve", lambda e, j=j: e.reduce_sum(out=dcol[:, j:j + 1], in_=dtmp[:], axis=AX.X),
                             reads=[dtmp.b], writes=[dcol.b])
                    P.op("dve", lambda e: e.tensor_copy(out=sh1c[:], in_=dcol[:, 0:8]), reads=[dcol.b], writes=[sh1c.b])
                    P.op("dve", lambda e: e.scalar_tensor_tensor(out=A1c[:], in0=dcol[:, 8:16], scalar=1.0, in1=gmc[:], op0=ALU.add, op1=ALU.mult),
                         reads=[dcol.b, gmc.b], writes=[A1c.b])
                    P.op("dve", lambda e: e.scalar_tensor_tensor(out=modB[:, 4 * D:5 * D], in0=modB[:, 4 * D:5 * D], scalar=1.0, in1=gffB[:],
                                                                 op0=ALU.add, op1=ALU.mult), reads=[modB.b, gffB.b], writes=[modB.b])
                    P.op("pool", lambda e: e.tensor_copy(out=lateB[:, 0, :], in_=modB[:, 5 * D:6 * D]), reads=[modB.b], writes=[lateB.b])
                    P.op("dve", lambda e: e.scalar_tensor_tensor(out=lateB[:, 1, :], in0=fmodB[:, D:2 * D], scalar=1.0, in1=gfinB[:],
                                                                 op0=ALU.add, op1=ALU.mult), reads=[fmodB.b, gfinB.b], writes=[lateB.b])
                    P.op("pool", lambda e: e.tensor_copy(out=lateB[:, 2, :], in_=fmodB[:, 0:D]), reads=[fmodB.b], writes=[lateB.b])

                    store("pool", modB, dr["mod_d"][:, :], modB[:, 2 * D:5 * D], [db["mod_d"]])
                    posi = sb(S0, "posi", [128, S // 4], I32)
                    ang = sb(S0, "ang", [128, S // 4], F32)
                    kf = sb(S0, "kf", [128, S // 4], F32)
                    ki = sb(S0, "ki", [128, S // 4], I32)
                    invf = sb(S0, "invf", [128, 1], F32)
                    for q in range(4):
                        load("sp", posi, posi[q * 32:(q + 1) * 32, :], dr["pos"][0, q * (S // 4):(q + 1) * (S // 4)].partition_broadcast(32))
                    load("sp", invf, invf[:], dr["invf"][:, :])
                    P.op("dve", lambda e: e.tensor_copy(out=kf[:], in_=posi[:]), reads=[posi.b], writes=[kf.b])
                    P.op("dve", lambda e: e.tensor_scalar(out=ang[:], in0=kf[:], scalar1=invf[:, 0:1], scalar2=None, op0=ALU.mult),
                         reads=[kf.b, invf.b], writes=[ang.b])
                    for which, shift in (("sin_d", 0.0), ("cos_d", PI / 2)):
                        if shift != 0.0:
                            P.op("dve", lambda e, shift=shift: e.tensor_scalar(out=ang[:], in0=ang[:], scalar1=shift, scalar2=None, op0=ALU.add),
                                 reads=[ang.b], writes=[ang.b])
                        P.op("dve", lambda e: e.tensor_scalar(out=kf[:], in0=ang[:], scalar1=1.0 / TWO_PI, scalar2=None, op0=ALU.mult),
                             reads=[ang.b], writes=[kf.b])
                        P.op("dve", lambda e: e.tensor_copy(out=ki[:], in_=kf[:]), reads=[kf.b], writes=[ki.b])
                        P.op("dve", lambda e: e.tensor_copy(out=kf[:], in_=ki[:]), reads=[ki.b], writes=[kf.b])
                        P.op("dve", lambda e: e.scalar_tensor_tensor(out=kf[:], in0=kf[:], scalar=-TWO_PI, in1=ang[:], op0=ALU.mult, op1=ALU.add),
                             reads=[kf.b, ang.b], writes=[kf.b])
                        P.op("dve", lambda e: e.tensor_scalar(out=posi[:].bitcast(F32), in0=kf[:], scalar1=PI, scalar2=TWO_PI, op0=ALU.is_gt, op1=ALU.mult),
                             reads=[kf.b], writes=[posi.b])
                        P.op("dve", lambda e: e.tensor_tensor(out=kf[:], in0=kf[:], in1=posi[:].bitcast(F32), op=ALU.subtract),
                             reads=[kf.b, posi.b], writes=[kf.b])
                        P.op("dve", lambda e: e.tensor_scalar(out=posi[:].bitcast(F32), in0=kf[:], scalar1=-PI, scalar2=TWO_PI, op0=ALU.is_lt, op1=ALU.mult),
                             reads=[kf.b], writes=[posi.b])
                        P.op("dve", lambda e: e.tensor_tensor(out=kf[:], in0=kf[:], in1=posi[:].bitcast(F32), op=ALU.add),
                             reads=[kf.b, posi.b], writes=[kf.b])
                        P.op("dve", lambda e: e.tensor_scalar(out=kf[:], in0=kf[:], scalar1=3.1415925, scalar2=-3.1415925, op0=ALU.min, op1=ALU.max),
                             reads=[kf.b], writes=[kf.b])
                        P.op("act", lambda e: e.activation(out=kf[:], in_=kf[:], func=AF.Sin), reads=[kf.b], writes=[kf.b])
                        for q in range(4):
                            for dup in range(2):
                                P.dma("pool", lambda e, which=which, q=q, dup=dup: e.dma_start(
                                    out=dr[which][dup * 32:(dup + 1) * 32, q * (S // 4):(q + 1) * (S // 4)], in_=kf[q * 32:(q + 1) * 32, :]),
                                    Buf("rst"), reads=[kf.b], writes=[db[which]])
                    P.barrier()
                    P.emit()
                if stop_after == 0:
                    return nc

                rmat = sb(SA, "rmat", [64, 64], BF16)
                rc0 = sb(SA, "rc0", [128, 64], F32)
                load("sp", rc0, rc0[:], dr["rc0"][0].partition_broadcast(128))
                ptmp = sb(SA, "ptmp", [128, 16], F32)
                psc = sb(SA, "psc", [128, 8], F32)
                gqc = sb(SA, "gqc", [128, 6], F32)
                gkc = sb(SA, "gkc", [128, 2], F32)
                load("sp", rmat, rmat[:], dr["rmat"][:, :])
                load("sp", psc, psc[:], dr["pscale"][:, :])
                load("sp", gqc, gqc[:], dr["g_q"][:, :])
                load("sp", gkc, gkc[:], dr["g_kv"][:, :])

                xt = [sb(SA, "xt%d" % i, [128, D], F32) for i in range(2)]
                xn = [sb(SA, "xn%d" % i, [128, D], BF16) for i in range(2)]
                ssq = [sb(SA, "ssq%d" % i, [128, 1], F32) for i in range(2)]
                hT = [sb(SA, "hT%d" % i, [128, 8, TW], BF16) for i in range(2)]
                uext = [sb(SA, "uext%d" % i, [128, 528], F32) for i in range(2)]
                s2 = sb(SA, "s2", [128, 528], F32)
                s4 = sb(SA, "s4", [128, 528], F32)
                s8 = sb(SA, "s8", [128, 528], F32)
                s16 = sb(SA, "s16", [128, 528], F32)
                halo = [sb(SA, "halo%d" % i, [128, 16], F32) for i in range(8)]
                mixed = sb(SA, "mixed", [128, 8, TW], BF16)
                yT = sb(SA, "yT", [128, 8, TW], BF16)
                aT = [sb(SA, "aT%d" % i, [128, TW], BF16) for i in range(2)]
                zq = sb(SA, "zq", [128, 6, TW], BF16)
                sqb = [sb(SA, "sqb%d" % i, [128, TW], BF16) for i in range(2)]
                rq = sb(SA, "rq", [128, TW], F32)
                qnT = sb(SA, "qnT", [128, 6, TW], BF16)
                qo = [sb(SA, "qo%d" % i, [128, TW], BF16) for i in range(2)]
                qpo = [sb(SA, "qpo%d" % i, [64, TW], BF16) for i in range(2)]
                ko = [sb(SA, "ko%d" % i, [128, TW], BF16) for i in range(2)]
                kpo = [sb(SA, "kpo%d" % i, [64, TW], BF16) for i in range(2)]
                vo = [sb(SA, "vo%d" % i, [128, D], BF16) for i in range(2)]
                abf = sb(SA, "abf", [64, TW], BF16)
                rt1 = sb(SA, "rt1", [64, TW], F32)
                rt2 = sb(SA, "rt2", [64, TW], F32)
                cosT = sb(SA, "cosT", [64, TW], F32)
                sinT = sb(SA, "sinT", [64, TW], F32)
                zkv = sb(SA, "zkv", [128, 2, TW], BF16)
                rkv = sb(SA, "rkv", [128, TW], F32)
                kvn = sb(SA, "kvn", [128, 2, TW], BF16)
                ptr = [ps(SA, "ptr%d" % i, [128, 8, 128], BF16) for i in range(2)]
                zps = [ps(SA, "zps%d" % i, [128, TW], F32) for i in range(4)]
                ssps = ps(SA, "ssps", [128, TW], F32)
                rps = ps(SA, "rps", [128, TW], F32)
                zi = [0]

                def nz():
                    zi[0] += 1
                    return zps[zi[0] % 4]

                for c in range(8):
                    P.op("pool", lambda e, c=c: e.memset(halo[c][:], 0.0), writes=[halo[c].b])

                def rstd_from(pssum, dst, n):
                    P.op("dve", lambda e: e.tensor_scalar(out=dst[:], in0=pssum[:], scalar1=1.0 / n, scalar2=EPS, op0=ALU.mult, op1=ALU.add),
                         reads=[pssum.b], writes=[dst.b])
                    P.op("act", lambda e: e.activation(out=dst[:], in_=dst[:], func=AF.Sqrt), reads=[dst.b], writes=[dst.b])
                    P.op("dve", lambda e: e.reciprocal(out=dst[:], in_=dst[:]), reads=[dst.b], writes=[dst.b])

                def rotary(pz, dst_ap, dstT, rows=64):
                    P.op("act", lambda e: e.copy(out=abf[:], in_=pz[0:64, :]), reads=[pz.b], writes=[abf.b])
                    P.op("pe", lambda e: e.matmul(rps[0:64, :], lhsT=rmat[:], rhs=abf[:], start=True, stop=True),
                         reads=[rmat.b, abf.b], writes=[rps.b])
                    P.op("dve", lambda e: e.tensor_tensor(out=rt1[:], in0=pz[0:64, :], in1=cosT[:], op=ALU.mult),
                         reads=[pz.b, cosT.b, abf.b], writes=[rt1.b])
                    P.op("dve", lambda e: e.tensor_tensor(out=rt2[:], in0=rps[0:64, :], in1=sinT[:], op=ALU.mult),
                         reads=[rps.b, sinT.b], writes=[rt2.b])
                    P.op("pool", lambda e: e.tensor_tensor(out=dst_ap, in0=rt1[:], in1=rt2[:], op=ALU.add),
                         reads=[rt1.b, rt2.b], writes=[dstT.b])

                def front(Tt):
                    hb = hT[Tt % 2]
                    for blk in range(4):
                        i = Tt * 4 + blk
                        xb_, xnb, sq_, pt_ = xt[i % 2], xn[i % 2], ssq[i % 2], ptr[i % 2]
                        load("sp", xb_, xb_[:], dr["x"][i * 128:(i + 1) * 128, :])
                        P.op("act", lambda e, xb_=xb_, sq_=sq_, xnb=xnb: e.activation(out=xnb[:], in_=xb_[:], func=AF.Square, accum_out=sq_[:]),
                             reads=[xb_.b], writes=[xnb.b, sq_.b])
                        rstd_from(sq_, sq_, D)
                        P.op("dve", lambda e, xb_=xb_, xnb=xnb, sq_=sq_: e.tensor_scalar(out=xnb[:], in0=xb_[:], scalar1=sq_[:, 0:1], scalar2=None, op0=ALU.mult),
                             reads=[xb_.b, sq_.b], writes=[xnb.b])

                        def tr(e, xnb=xnb, pt_=pt_):
                            for k in range(8):
                                ins = e.transpose(out=pt_[:, k, :], in_=xnb[:, k * 128:(k + 1) * 128], identity=identb[:])
                            return ins
                        P.op("pe", tr, reads=[xnb.b, identb.b], writes=[pt_.b])

                        def ev(e, pt_=pt_, hb=hb, blk=blk, ks=(0, 1, 2, 3)):
                            for k in ks:
                                ins = e.activation(out=hb[:, k, blk * 128:(blk + 1) * 128], in_=pt_[:, k, :], func=AF.Identity,
                                                   scale=A1c[:, k:k + 1], bias=sh1c[:, k:k + 1])
                            return ins

                        def ev2(e, pt_=pt_, hb=hb, blk=blk, ks=(4, 5, 6, 7)):
                            for k in ks:
                                ins = e.tensor_scalar(out=hb[:, k, blk * 128:(blk + 1) * 128], in0=pt_[:, k, :],
                                                      scalar1=A1c[:, k:k + 1], scalar2=sh1c[:, k:k + 1], op0=ALU.mult, op1=ALU.add)
                            return ins
                        if "ev" not in SKIP:
                            P.op("act", ev, reads=[pt_.b, A1c.b, sh1c.b], writes=[hb.b])
                        if "ev2" not in SKIP:
                            P.op("dve", ev2, reads=[pt_.b, A1c.b, sh1c.b], writes=[hb.b])
                    store("sp", hb, dr["h_d"][Tt], hb[:], [db["h_d"][Tt]])

                front(0)
                for Tt in range(NT):
                    t0 = Tt * TW
                    hb = hT[Tt % 2]
                    load("sp", cosT, cosT[:], dr["cos_d"][:, t0:t0 + TW], [db["cos_d"]])
                    load("sp", sinT, sinT[:], dr["sin_d"][:, t0:t0 + TW], [db["sin_d"]])

                    def zmm(pz, c0, m=128, hb=hb):
                        def f(e, hb=hb):
                            for k in range(8):
                                ins = e.matmul(pz[0:m, :], lhsT=wina[:, k, c0:c0 + m], rhs=hb[:, k, :], start=(k == 0), stop=(k == 7))
                            return ins
                        P.op("pe", f, reads=[wina.b, hb.b], writes=[pz.b])

                    for c in range(8):
                        g = c // 2
                        ue = uext[c % 2]
                        pz = nz()
                        zmm(pz, c * 128)
                        P.op("pool", lambda e, ue=ue, c=c: e.tensor_copy(out=ue[:, 1:16], in_=halo[c][:, 1:16]), reads=[halo[c].b], writes=[ue.b])
                        P.op("act", lambda e, ue=ue, pz=pz: e.copy(out=ue[:, 16:528], in_=pz[:]), reads=[pz.b], writes=[ue.b])
                        P.op("pool", lambda e, ue=ue, c=c: e.tensor_copy(out=halo[c][:, 1:16], in_=ue[:, 513:528]), reads=[ue.b], writes=[halo[c].b])
                        chain = [(s2, 2, 1), (s4, 4, 2), (s8, 8, 4), (s16, 16, 8)][:g + 1]
                        prev = ue
                        for (sx, lo, sh) in chain:
                            P.op("dve", lambda e, sx=sx, lo=lo, sh=sh, prev=prev: e.tensor_tensor(
                                out=sx[:, lo:528], in0=prev[:, lo:528], in1=prev[:, lo - sh:528 - sh], op=ALU.add),
                                reads=[prev.b], writes=[sx.b])
                            prev = sx
                        w = 2 ** (g + 1)
                        P.op("dve", lambda e, prev=prev, ue=ue, c=c, w=w: e.scalar_tensor_tensor(
                            out=mixed[:, c, :], in0=prev[:, 16:528], scalar=1.0 / w, in1=ue[:, 16:528], op0=ALU.mult, op1=ALU.subtract),
                            reads=[prev.b, ue.b], writes=[mixed.b])
                        if Tt == 0:
                            P.op("dve", lambda e, prev=prev, g=g: e.tensor_tensor(out=ptmp[:], in0=prev[:, 16:32], in1=rc0[:, g * 16:(g + 1) * 16], op=ALU.mult),
                                 reads=[prev.b, rc0.b], writes=[ptmp.b])
                            P.op("dve", lambda e, ue=ue, c=c: e.tensor_tensor(out=mixed[:, c, 0:16], in0=ptmp[:], in1=ue[:, 16:32], op=ALU.subtract),
                                 reads=[ptmp.b, ue.b], writes=[mixed.b])
                    def latent1(nch, col0, ztile, gcol, pss):
                        for c in range(nch):
                            pz = nz()
                            zmm(pz, col0 + c * 128)
                            sq_ = sqb[c % 2]
                            P.op("act", lambda e, pz=pz, sq_=sq_: e.activation(out=sq_[:], in_=pz[:], func=AF.Square), reads=[pz.b], writes=[sq_.b])
                            P.op("act", lambda e, pz=pz, c=c: e.activation(out=ztile[:, c, :], in_=pz[:], func=AF.Identity, scale=gcol[:, c:c + 1]),
                                 reads=[pz.b, gcol.b], writes=[ztile.b])
                            P.op("pe", lambda e, sq_=sq_, c=c: e.matmul(pss[:], lhsT=onesb[:], rhs=sq_[:], start=(c == 0), stop=(c == nch - 1)),
                                 reads=[onesb.b, sq_.b], writes=[pss.b])

                    def latent2(nch, ztile, rdst, ndst, nfeat, pss):
                        rstd_from(pss, rdst, nfeat)
                        for c in range(nch):
                            P.op("dve", lambda e, c=c: e.tensor_tensor(out=ndst[:, c, :], in0=ztile[:, c, :], in1=rdst[:], op=ALU.mult),
                                 reads=[ztile.b, rdst.b], writes=[ndst.b])
                    latent1(6, 1024, zq, gqc, ssps)
                    latent1(2, 1792, zkv, gkc, rps)
                    latent2(6, zq, rq, qnT, 768, ssps)
                    latent2(2, zkv, rkv, kvn, 256, rps)
                    for g in range(4):
                        for mo in range(2):
                            pz = nz()

                            def f(e, pz=pz, g=g, mo=mo):
                                for kk in range(2):
                                    ins = e.matmul(pz[:], lhsT=wgrp[:, g, kk, mo * 128:(mo + 1) * 128], rhs=mixed[:, 2 * g + kk, :],
                                                   start=(kk == 0), stop=(kk == 1))
                                return ins
                            P.op("pe", f, reads=[wgrp.b, mixed.b], writes=[pz.b])
                            cc = 2 * g + mo
                            P.op("act", lambda e, pz=pz, cc=cc: e.activation(out=yT[:, cc, :], in_=pz[:], func=AF.Identity, scale=psc[:, cc:cc + 1]),
                                 reads=[pz.b, psc.b], writes=[yT.b])
                    for mo in range(8):
                        pz = nz()
                        ab = aT[mo % 2]

                        def f(e, pz=pz, mo=mo):
                            for k in range(8):
                                ins = e.matmul(pz[:], lhsT=wproj[:, k, mo * 128:(mo + 1) * 128], rhs=yT[:, k, :], start=(k == 0), stop=(k == 7))
                            return ins
                        P.op("pe", f, reads=[wproj.b, yT.b], writes=[pz.b])
                        eng = "act" if mo % 2 == 0 else "dve"
                        if eng == "act":
                            P.op("act", lambda e, pz=pz, mo=mo, ab=ab: e.copy(out=ab[:], in_=pz[:]), reads=[pz.b], writes=[ab.b])
                        else:
                            P.op("dve", lambda e, pz=pz, mo=mo, ab=ab: e.tensor_copy(out=ab[:], in_=pz[:]), reads=[pz.b], writes=[ab.b])
                        store("sp", ab, dr["a_d"][Tt][:, mo, :], ab[:], [db["a_d"][Tt]])

                    if Tt + 1 < NT:
                        front(Tt + 1)
                    for h in range(8):
                        pz = nz()
                        qob = qo[h % 2]

                        def f(e, pz=pz, h=h):
                            for k in range(6):
                                ins = e.matmul(pz[:], lhsT=wq[:, k, h * 128:(h + 1) * 128], rhs=qnT[:, k, :], start=(k == 0), stop=(k == 5))
                            return ins
                        P.op("pe", f, reads=[wq.b, qnT.b], writes=[pz.b])
                        P.op("act", lambda e, pz=pz, h=h, qob=qob: e.copy(out=qob[:], in_=pz[:]), reads=[pz.b], writes=[qob.b])
                        store("sp", qob, dr["qn_d"][h, :, t0:t0 + TW], qob[:], [db["qn_d"][Tt]])
                    for h in range(8):
                        pz = nz()
                        qpb = qpo[h % 2]

                        def f(e, pz=pz, h=h):
                            for k in range(6):
                                ins = e.matmul(pz[0:64, :], lhsT=wq[:, k, 1024 + h * 64:1024 + (h + 1) * 64], rhs=qnT[:, k, :], start=(k == 0), stop=(k == 5))
                            return ins
                        P.op("pe", f, reads=[wq.b, qnT.b], writes=[pz.b])
                        rotary(pz, qpb[:], qpb)
                        store("sp", qpb, dr["qp_d"][h, :, t0:t0 + TW], qpb[:], [db["qp_d"][Tt]])

                    kpb = kpo[Tt % 2]
                    for h in range(8):
                        pz = nz()
                        kob = ko[h % 2]

                        def f(e, pz=pz, h=h):
                            for k in range(2):
                                ins = e.matmul(pz[:], lhsT=wkv[:, k, h * 128:(h + 1) * 128], rhs=kvn[:, k, :], start=(k == 0), stop=(k == 1))
                            return ins
                        P.op("pe", f, reads=[wkv.b, kvn.b], writes=[pz.b])
                        P.op("dve", lambda e, pz=pz, h=h, kob=kob: e.tensor_copy(out=kob[:], in_=pz[:]), reads=[pz.b], writes=[kob.b])
                        store("sp", kob, dr["kn_d"][h, :, t0:t0 + TW], kob[:], [db["kn_d"][Tt]])
                    for blk in range(4):
                        i = Tt * 4 + blk
                        vb = vo[i % 2]
                        for j in range(2):
                            pz = nz()

                            def f(e, pz=pz, blk=blk, j=j):
                                for k in range(2):
                                    ins = e.matmul(pz[:], lhsT=kvn[:, k, blk * 128:(blk + 1) * 128], rhs=wkv[:, k, 1024 + j * 512:1024 + (j + 1) * 512],
                                                   start=(k == 0), stop=(k == 1))
                                return ins
                            P.op("pe", f, reads=[wkv.b, kvn.b], writes=[pz.b])
                            P.op("act", lambda e, pz=pz, j=j, vb=vb: e.copy(out=vb[:, j * 512:(j + 1) * 512], in_=pz[:]), reads=[pz.b], writes=[vb.b])
                        store("sp", vb, dr["v_d"][i], vb[:], [db["v_d"][i]])
                    pz = nz()
                    zmm(pz, 2048, 64)
                    rotary(pz, kpb[:], kpb)
                    store("sp", kpb, dr["kp_d"][:, t0:t0 + TW], kpb[:], [db["kp_d"]])
                P.barrier()
                P.emit()
            if stop_after == 1:
                return nc

            with ExitStack() as SB:
                kc = sb(SB, "kc", [128, 8, S], BF16)
                kpc = sb(SB, "kpc", [128, S], BF16)
                vaug = sb(SB, "vaug", [128, NB, 8, 129], BF16)
                qt = [sb(SB, "qt0", [128, 8, TW], BF16)] * 2
                qpt = [sb(SB, "qpt0", [128, 8, TW], BF16)] * 2
                pT = [sb(SB, "pT%d" % i, [128, TW], BF16) for i in range(3)]
                otok = sb(SB, "otok", [128, 4, D], BF16)
                oT = [sb(SB, "oT0", [128, 8, TW], BF16)] * 2
                rs = sb(SB, "rs", [128, 4], F32)
                sps = [ps(SB, "sps%d" % i, [128, TW], F32) for i in range(3)]
                ops_ = [ps(SB, "ops%d" % i, [128, 512], F32) for i in range(4)]
                pto = ps(SB, "pto", [128, 8, 128], BF16)
                kcb = [Buf("kc%d" % i) for i in range(NT)]
                vb_ = [Buf("va%d" % i) for i in range(NB)]
                vones = Buf("vones")
                P.op("pool", lambda e: e.memset(vaug[:, :, :, 128:129], 1.0), writes=[vones])
                kpz = Buf("kpz")
                P.op("pool", lambda e: e.memset(kpc[64:128, :], 0.0), writes=[kpz])
                P.op("pool", lambda e: e.memset(qpt[0][64:128, :, :], 0.0), writes=[kpz])
                si = [0]
                for Tt in range(NT):
                    t0 = Tt * TW
                    P.dma("sp", lambda e, t0=t0: e.dma_start(out=kc[:, :, t0:t0 + TW], in_=dr["kn_d"][:, :, t0:t0 + TW].rearrange("h p t -> p h t")),
                          kcb[Tt], reads=[db["kn_d"][Tt]], writes=[kcb[Tt]])
                    P.dma("sp", lambda e, t0=t0: e.dma_start(out=kpc[0:64, t0:t0 + TW], in_=dr["kp_d"][:, t0:t0 + TW]),
                          kcb[Tt], reads=[db["kp_d"]], writes=[kcb[Tt]])
                    for blk in range(4):
                        i = Tt * 4 + blk
                        P.dma("sp", lambda e, i=i: e.dma_start(out=vaug[:, i, :, 0:128], in_=dr["v_d"][i].rearrange("p (h d) -> p h d", h=8)),
                              vb_[i], reads=[db["v_d"][i]], writes=[vb_[i]])
                    qb, qpb = qt[Tt % 2], qpt[Tt % 2]
                    load("sp", qb, qb[:], dr["qn_d"][:, :, t0:t0 + TW].rearrange("h p t -> p h t"), [db["qn_d"][Tt]])
                    load("sp", qpb, qpb[0:64, :, :], dr["qp_d"][:, :, t0:t0 + TW].rearrange("h p t -> p h t"), [db["qp_d"][Tt]])
                    nkb = 4 * Tt + 4
                    for h in range(8):
                        pend = None
                        for kb in range(nkb + 1):
                            if kb < nkb:
                                c0 = max(0, (kb - 4 * Tt)) * 128
                                sp_ = sps[si[0] % 3]
                                pt_ = pT[si[0] % 3]
                                si[0] += 1

                                def qk(e, sp_=sp_, kb=kb, c0=c0, h=h, qb=qb, qpb=qpb):
                                    e.matmul(sp_[:, c0:TW], lhsT=kc[:, h, kb * 128:(kb + 1) * 128], rhs=qb[:, h, c0:TW], start=True, stop=False)
                                    return e.matmul(sp_[:, c0:TW], lhsT=kpc[:, kb * 128:(kb + 1) * 128], rhs=qpb[:, h, c0:TW], start=False, stop=True)
                                P.op("pe", qk, reads=[kcb[kb // 4], qb.b, qpb.b, kpz], writes=[sp_.b])
                                P.op("act", lambda e, sp_=sp_, pt_=pt_, c0=c0: e.activation(out=pt_[:, c0:TW], in_=sp_[:, c0:TW], func=AF.Exp, scale=SM_SCALE),
                                     reads=[sp_.b], writes=[pt_.b])
                                if kb >= 4 * Tt:
                                    P.op("pool", lambda e, pt_=pt_, c0=c0: e.memset(pt_[64:128, c0:c0 + 64], 0.0), writes=[pt_.b])
                                cur = (kb, pt_, c0)
                            else:
                                cur = None
                            if pend is not None:
                                kbp, ptp, c0p = pend

                                def pv(e, kbp=kbp, ptp=ptp, c0p=c0p, h=h, Tt=Tt):
                                    ins = None
                                    for ql in range(c0p // 128, 4):
                                        qi = 4 * Tt + ql
                                        ins = e.matmul(ops_[ql][:, 0:129], lhsT=ptp[:, ql * 128:(ql + 1) * 128], rhs=vaug[:, kbp, h, :],
                                                       start=(kbp == 0), stop=(kbp == qi))
                                    return ins
                                P.op("pe", pv, reads=[ptp.b, vb_[kbp], vones], writes=[o.b for o in ops_[c0p // 128:]])
                                if kbp >= 4 * Tt:
                                    ql = kbp - 4 * Tt
                                    P.op("dve", lambda e, ql=ql: e.reciprocal(out=rs[:, ql:ql + 1], in_=ops_[ql][:, 128:129]),
                                         reads=[ops_[ql].b], writes=[rs.b])
                                    P.op("dve", lambda e, ql=ql, h=h: e.tensor_scalar(out=otok[:, ql, h * 128:(h + 1) * 128], in0=ops_[ql][:, 0:128],
                                                                                     scalar1=rs[:, ql:ql + 1], scalar2=None, op0=ALU.mult),
                                         reads=[ops_[ql].b, rs.b], writes=[otok.b])
                            pend = cur
                    ob = oT[Tt % 2]
                    for ql in range(4):
                        def tr(e, ql=ql):
                            for k in range(8):
                                ins = e.transpose(out=pto[:, k, :], in_=otok[:, ql, k * 128:(k + 1) * 128], identity=identb[:])
                            return ins
                        P.op("pe", tr, reads=[otok.b, identb.b], writes=[pto.b])
                        P.op("act", lambda e, ql=ql, ob=ob: e.copy(out=ob[:, :, ql * 128:(ql + 1) * 128], in_=pto[:]), reads=[pto.b], writes=[ob.b])
                    store("pool", ob, dr["o_d"][Tt], ob[:], [db["o_d"][Tt]])
                P.barrier()
                P.emit()
            if stop_after == 2:
                return nc

            with ExitStack() as SC:
                wing = sb(SC, "wing", [128, 8, 2048], BF16)
                wmo = sb(SC, "wmo", [128, 8, D], BF16)
                wout = sb(SC, "wout", [128, 8, D], BF16)
                wr = sb(SC, "wr", [128, 8, NE], F32)
                brB = sb(SC, "brB", [128, NE], F32)
                bgc = sb(SC, "bgc", [128, 16], F32)
                for k in range(8):
                    load("pool", wing, wing[:, k, :], dr["w_in"][k * 128:(k + 1) * 128, 2112:4160], max_dma_last_dim=4096)
                    load("pool", wmo, wmo[:, k, :], dr["w_mo"][k * 128:(k + 1) * 128, :])
                    load("pool", wout, wout[:, k, :], dr["w_out"][k * 128:(k + 1) * 128, :])
                load("sp", wr, wr[:], dr["w_r"].rearrange("(k p) n -> p k n", p=128))
                load("sp", brB, brB[:], dr["b_r"][0].partition_broadcast(128))
                load("sp", bgc, bgc[:], dr["b_gate"][:, :])
                hb = [sb(SC, "hb0", [128, 8, TW], BF16)] * 2
                ab = [sb(SC, "ab0", [128, 8, TW], BF16)] * 2
                ob = [sb(SC, "ob0", [128, 8, TW], BF16)] * 2
                modC = sb(SC, "modC", [128, 3 * D], F32)
                load("sp", modC, modC[:], dr["mod_d"][:, :], [db["mod_d"]])
                gA = sb(SC, "gA", [128, TW], F32)
                gB = sb(SC, "gB", [128, TW], F32)
                mt1 = sb(SC, "mt1", [128, TW], F32)
                mt2 = sb(SC, "mt2", [128, TW], F32)
                merged = sb(SC, "merged", [128, 8, TW], BF16)
                xt = [sb(SC, "xtc%d" % i, [128, D], F32) for i in range(2)]
                x1t = [sb(SC, "x1t%d" % i, [128, D], F32) for i in range(2)]
                tmpc = sb(SC, "tmpc", [128, D], F32)
                h2 = sb(SC, "h2", [128, D], F32)
                junk = sb(SC, "junkc", [128, D], BF16)
                ss2 = sb(SC, "ss2", [128, 1], F32)
                h2Tf = sb(SC, "h2Tf", [128, 8, 128], F32)
                h2bf = [sb(SC, "h2bf%d" % i, [128, D], BF16) for i in range(2)]
                lg = sb(SC, "lg", [128, NE], F32)
                m8 = sb(SC, "m8", [128, 8], F32)
                nm = sb(SC, "nm", [128, 1], F32)
                msk = sb(SC, "msk", [128, NE], F32)
                ex = sb(SC, "ex", [128, NE], F32)
                esum = sb(SC, "esum", [128, 1], F32)
                psA = ps(SC, "psA", [128, TW], F32)
                psB = ps(SC, "psB", [128, TW], F32)
                psM = ps(SC, "psM", [128, TW], F32)
                psX = [ps(SC, "psX%d" % i, [128, 512], F32) for i in range(2)]
                psT = [ps(SC, "psT%d" % i, [128, 4, 128], F32) for i in range(2)]
                psR = ps(SC, "psR", [128, 512], F32)

                def rstd2(src, dst, n):
                    P.op("dve", lambda e: e.tensor_scalar(out=dst[:], in0=src[:], scalar1=1.0 / n, scalar2=EPS, op0=ALU.mult, op1=ALU.add),
                         reads=[src.b], writes=[dst.b])
                    P.op("act", lambda e: e.activation(out=dst[:], in_=dst[:], func=AF.Sqrt), reads=[dst.b], writes=[dst.b])
                    P.op("dve", lambda e: e.reciprocal(out=dst[:], in_=dst[:]), reads=[dst.b], writes=[dst.b])

                for Tt in range(NT):
                    h_, a_, o_ = hb[Tt % 2], ab[Tt % 2], ob[Tt % 2]
                    load("sp", h_, h_[:], dr["h_d"][Tt], [db["h_d"][Tt]])
                    load("sp", a_, a_[:], dr["a_d"][Tt], [db["a_d"][Tt]])
                    load("sp", o_, o_[:], dr["o_d"][Tt], [db["o_d"][Tt]])
                    for mo in range(8):
                        def fa(e, mo=mo, h_=h_):
                            for k in range(8):
                                ins = e.matmul(psA[:], lhsT=wing[:, k, mo * 128:(mo + 1) * 128], rhs=h_[:, k, :], start=(k == 0), stop=(k == 7))
                            return ins

                        def fb(e, mo=mo, h_=h_):
                            for k in range(8):
                                ins = e.matmul(psB[:], lhsT=wing[:, k, 1024 + mo * 128:1024 + (mo + 1) * 128], rhs=h_[:, k, :], start=(k == 0), stop=(k == 7))
                            return ins

                        def fm(e, mo=mo, o_=o_):
                            for k in range(8):
                                ins = e.matmul(psM[:], lhsT=wmo[:, k, mo * 128:(mo + 1) * 128], rhs=o_[:, k, :], start=(k == 0), stop=(k == 7))
                            return ins
                        P.op("pe", fa, reads=[wing.b, h_.b], writes=[psA.b])
                        P.op("pe", fb, reads=[wing.b, h_.b], writes=[psB.b])
                        P.op("pe", fm, reads=[wmo.b, o_.b], writes=[psM.b])
                        P.op("act", lambda e, mo=mo: e.activation(out=gA[:], in_=psA[:], func=AF.Sigmoid, bias=bgc[:, mo:mo + 1]),
                             reads=[psA.b, bgc.b], writes=[gA.b])
                        P.op("act", lambda e, mo=mo: e.activation(out=gB[:], in_=psB[:], func=AF.Sigmoid, bias=bgc[:, 8 + mo:9 + mo]),
                             reads=[psB.b, bgc.b], writes=[gB.b])
                        P.op("pool", lambda e, mo=mo, a_=a_: e.tensor_tensor(out=mt1[:], in0=gA[:], in1=a_[:, mo, :], op=ALU.mult),
                             reads=[gA.b, a_.b], writes=[mt1.b])
                        P.op("dve", lambda e: e.tensor_tensor(out=mt2[:], in0=psM[:], in1=gB[:], op=ALU.mult),
                             reads=[psM.b, gB.b], writes=[mt2.b])
                        P.op("dve", lambda e, mo=mo: e.tensor_tensor(out=merged[:, mo, :], in0=mt1[:], in1=mt2[:], op=ALU.add),
                             reads=[mt1.b, mt2.b], writes=[merged.b])
                    def fx_emit(blk):
                        for j in range(2):
                            def fx(e, blk=blk, j=j):
                                for k in range(8):
                                    ins = e.matmul(psX[j][:], lhsT=merged[:, k, blk * 128:(blk + 1) * 128], rhs=wout[:, k, j * 512:(j + 1) * 512],
                                                   start=(k == 0), stop=(k == 7))
                                return ins
                            P.op("pe", fx, reads=[merged.b, wout.b], writes=[psX[j].b])
                    fx_emit(0)
                    for blk in range(4):
                        i = Tt * 4 + blk
                        xb_, x1b = xt[i % 2], x1t[i % 2]
                        load("sp", xb_, xb_[:], dr["x"][i * 128:(i + 1) * 128, :])
                        for j in range(2):
                            P.op("dve", lambda e, j=j: e.tensor_tensor(out=tmpc[:, j * 512:(j + 1) * 512], in0=psX[j][:], in1=modC[:, j * 512:(j + 1) * 512], op=ALU.mult),
                                 reads=[psX[j].b, modC.b], writes=[tmpc.b])
                        P.op("dve", lambda e, xb_=xb_, x1b=x1b: e.tensor_tensor(out=x1b[:], in0=tmpc[:], in1=xb_[:], op=ALU.add),
                             reads=[tmpc.b, xb_.b], writes=[x1b.b])
                        store("pool", x1b, dr["x1_d"][i], x1b[:], [db["x1_d"][i]])
                        P.op("act", lambda e, x1b=x1b: e.activation(out=junk[:], in_=x1b[:], func=AF.Square, accum_out=ss2[:]),
                             reads=[x1b.b], writes=[junk.b, ss2.b])
                        rstd2(ss2, ss2, D)
                        P.op("dve", lambda e, x1b=x1b: e.scalar_tensor_tensor(out=tmpc[:], in0=x1b[:], scalar=ss2[:, 0:1], in1=modC[:, 2 * D:3 * D], op0=ALU.mult, op1=ALU.mult),
                             reads=[x1b.b, ss2.b, modC.b], writes=[tmpc.b])
                        P.op("dve", lambda e: e.tensor_tensor(out=h2[:], in0=tmpc[:], in1=modC[:, D:2 * D], op=ALU.add),
                             reads=[tmpc.b, modC.b], writes=[h2.b])

                        if blk + 1 < 4:
                            fx_emit(blk + 1)
                        hbf = h2bf[i % 2]
                        P.op("act", lambda e, hbf=hbf: e.copy(out=hbf[:], in_=h2[:]), reads=[h2.b], writes=[hbf.b])
                        store("pool", hbf, dr["h2tok_d"][i * 128:(i + 1) * 128, :], hbf[:], [db["h2tok_d"][i]])

                        def trf(e):
                            for k in range(8):
                                ins = e.transpose(out=psT[k // 4][:, k % 4, :], in_=h2[:, k * 128:(k + 1) * 128], identity=identf[:])
                            return ins
                        P.op("pe", trf, reads=[h2.b, identf.b], writes=[psT[0].b, psT[1].b])
                        for hh in range(2):
                            P.op("act", lambda e, hh=hh: e.copy(out=h2Tf[:, hh * 4:(hh + 1) * 4, :], in_=psT[hh][:]), reads=[psT[hh].b], writes=[h2Tf.b])

                        def fr(e):
                            for k in range(8):
                                ins = e.matmul(psR[:, 0:NE], lhsT=h2Tf[:, k, :], rhs=wr[:, k, :], start=(k == 0), stop=(k == 7))
                            return ins
                        P.op("pe", fr, reads=[h2Tf.b, wr.b], writes=[psR.b])
                        P.op("dve", lambda e: e.tensor_tensor(out=lg[:], in0=psR[:, 0:NE], in1=brB[:], op=ALU.add), reads=[psR.b, brB.b], writes=[lg.b])
                        P.op("dve", lambda e: e.max(out=m8[:], in_=lg[:]), reads=[lg.b], writes=[m8.b])
                        P.op("dve", lambda e: e.tensor_scalar(out=nm[:], in0=m8[:, 0:1], scalar1=-1.0, scalar2=None, op0=ALU.mult), reads=[m8.b], writes=[nm.b])
                        P.op("dve", lambda e: e.tensor_scalar(out=msk[:], in0=lg[:], scalar1=m8[:, 3:4], scalar2=None, op0=ALU.is_ge), reads=[lg.b, m8.b], writes=[msk.b])
                        P.op("act", lambda e: e.activation(out=ex[:], in_=lg[:], func=AF.Exp, bias=nm[:, 0:1]), reads=[lg.b, nm.b], writes=[ex.b])
                        P.op("dve", lambda e: e.tensor_tensor(out=ex[:], in0=ex[:], in1=msk[:], op=ALU.mult), reads=[ex.b, msk.b], writes=[ex.b])
                        P.op("dve", lambda e: e.reduce_sum(out=esum[:], in_=ex[:], axis=AX.X), reads=[ex.b], writes=[esum.b])
                        P.op("dve", lambda e: e.reciprocal(out=esum[:], in_=esum[:]), reads=[esum.b], writes=[esum.b])
                        P.op("dve", lambda e, i=i: e.tensor_scalar(out=wdense[:, i, :], in0=ex[:], scalar1=esum[:, 0:1], scalar2=None, op0=ALU.mult),
                             reads=[ex.b, esum.b], writes=[wdense.b])
                if "wd_d" in dbg:
                    store("pool", wdense, dr["wd_d"][:, :], wdense[:].rearrange("p a b -> p (a b)"), [db["wd_d"]])
                P.barrier()
                P.emit()
        if stop_after == 3:
            return nc

        destI = sb(G, "destI", [128, 4, NB], I32)
        wkA = sb(G, "wkA", [128, 4, NB], F32)
        tbl = sb(G, "tbl", [128, 2, 4], I32)
        endI = sb(G, "endI", [128, NE], I32)
        with ExitStack() as SR:
            mskA = sb(SR, "mskA", [128, NB, NE], F32)
            mskAb = sb(SR, "mskAb", [128, NB * NE], BF16)
            utri = sb(SR, "utri", [128, 128], BF16)
            bcol = sb(SR, "bcol", [128, 2], F32)
            parB = sb(SR, "parB", [128, 2, NE], F32)
            posA = sb(SR, "posA", [128, NB, NE], F32)
            totA = sb(SR, "totA", [128, NB, NE], F32)
            cumB = sb(SR, "cumB", [128, NB, NE], F32)
            rk = [sb(SR, "rk%d" % i, [128, NB, NE], F32) for i in range(2)]
            selT = sb(SR, "selT", [128, NB, NE], F32)
            prodT = sb(SR, "prodT", [128, NB, NE], F32)
            destF = sb(SR, "destF", [128, 4, NB], F32)
            cnt = sb(SR, "cnt", [128, NE], F32)
            qv = sb(SR, "qv", [128, NE], F32)
            qi = sb(SR, "qi", [128, NE], I32)
            qf = sb(SR, "qf", [128, NE], F32)
            nbk = sb(SR, "nbk", [128, NE], F32)
            cs = [sb(SR, "cs%d" % i, [128, NE], F32) for i in range(2)]
            pst = sb(SR, "pst", [128, NE], F32)
            endS = sb(SR, "endS", [128, NE], F32)
            ele = sb(SR, "ele", [128, NE], F32)
            elp = sb(SR, "elp", [128, NE], F32)
            tblF = sb(SR, "tblF", [128, 2, 4], F32)
            nd = sb(SR, "nd", [128, 2], F32)
            psW = [ps(SR, "psW%d" % j, [128, 512], F32) for j in range(2)]
            psTt = [ps(SR, "psTt%d" % j, [128, 512], F32) for j in range(2)]
            load("sp", utri, utri[:], dr["utri"][:, :])
            load("sp", bcol, bcol[:], dr["bcol"][:, :])
            load("sp", parB, parB[:].rearrange("p a b -> p (a b)"), dr["par"][0].partition_broadcast(128))
            fl = lambda t_: t_[:].rearrange("p a b -> p (a b)")
            P.op("dve", lambda e: e.tensor_scalar(out=fl(mskA), in0=fl(wdense), scalar1=0.0, scalar2=None, op0=ALU.is_gt), reads=[wdense.b], writes=[mskA.b])
            P.op("dve", lambda e: e.tensor_copy(out=mskAb[:], in_=fl(mskA)), reads=[mskA.b], writes=[mskAb.b])
            for j in range(2):
                P.op("pe", lambda e, j=j: e.matmul(psW[j][:], lhsT=utri[:], rhs=mskAb[:, j * 512:(j + 1) * 512], start=True, stop=True),
                     reads=[utri.b, mskAb.b], writes=[psW[j].b])
                P.op("pe", lambda e, j=j: e.matmul(psTt[j][:], lhsT=onesb[:], rhs=mskAb[:, j * 512:(j + 1) * 512], start=True, stop=True),
                     reads=[onesb.b, mskAb.b], writes=[psTt[j].b])
                P.op("dve", lambda e, j=j: e.tensor_copy(out=fl(posA)[:, j * 512:(j + 1) * 512], in_=psW[j][:]), reads=[psW[j].b], writes=[posA.b])
                P.op("dve", lambda e, j=j: e.tensor_copy(out=fl(totA)[:, j * 512:(j + 1) * 512], in_=psTt[j][:]), reads=[psTt[j].b], writes=[totA.b])
            T0 = P.tick["pe"]
            P.op("dve", lambda e: e.memset(cumB[:, 0, :], 0.0), writes=[cumB.b])

            for i in range(1, NB):
                P.op("dve", lambda e, i=i: e.tensor_tensor(out=cumB[:, i, :], in0=cumB[:, i - 1, :], in1=totA[:, i - 1, :], op=ALU.add),
                     reads=[cumB.b, totA.b], writes=[cumB.b])
            P.op("dve", lambda e: e.tensor_tensor(out=cnt[:], in0=cumB[:, NB - 1, :], in1=totA[:, NB - 1, :], op=ALU.add), reads=[cumB.b, totA.b], writes=[cnt.b])
            P.op("dve", lambda e: e.tensor_scalar(out=qv[:], in0=cnt[:], scalar1=1.0 / 128, scalar2=None, op0=ALU.mult), reads=[cnt.b], writes=[qv.b])
            P.op("dve", lambda e: e.tensor_copy(out=qi[:], in_=qv[:]), reads=[qv.b], writes=[qi.b])
            P.op("dve", lambda e: e.tensor_copy(out=qf[:], in_=qi[:]), reads=[qi.b], writes=[qf.b])
            P.op("dve", lambda e: e.tensor_tensor(out=nbk[:], in0=qv[:], in1=qf[:], op=ALU.is_gt), reads=[qv.b, qf.b], writes=[nbk.b])
            P.op("dve", lambda e: e.tensor_tensor(out=nbk[:], in0=nbk[:], in1=qf[:], op=ALU.add), reads=[nbk.b, qf.b], writes=[nbk.b])
            P.op("dve", lambda e: e.tensor_copy(out=cs[0][:], in_=nbk[:]), reads=[nbk.b], writes=[cs[0].b])
            cur = 0
            for s_ in (1, 2, 4, 8, 16):
                a_, b_ = cs[cur], cs[1 - cur]
                P.op("dve", lambda e, a_=a_, b_=b_: e.tensor_copy(out=b_[:], in_=a_[:]), reads=[a_.b], writes=[b_.b])
                P.op("dve", lambda e, a_=a_, b_=b_, s_=s_: e.tensor_tensor(out=b_[:, s_:NE], in0=a_[:, s_:NE], in1=a_[:, 0:NE - s_], op=ALU.add),
                     reads=[a_.b], writes=[b_.b])
                cur = 1 - cur
            endb = cs[cur]
            P.op("dve", lambda e: e.tensor_tensor(out=pst[:], in0=endb[:], in1=nbk[:], op=ALU.subtract), reads=[endb.b, nbk.b], writes=[pst.b])
            P.op("dve", lambda e: e.tensor_scalar(out=pst[:], in0=pst[:], scalar1=128.0, scalar2=None, op0=ALU.mult), reads=[pst.b], writes=[pst.b])
            P.op("dve", lambda e: e.tensor_tensor(out=fl(posA), in0=fl(posA), in1=fl(cumB), op=ALU.add), reads=[posA.b, cumB.b], writes=[posA.b])

            def addp(e):
                for i in range(NB):
                    ins = e.tensor_tensor(out=posA[:, i, :], in0=posA[:, i, :], in1=pst[:], op=ALU.add)
                return ins
            P.op("dve", addp, reads=[posA.b, pst.b], writes=[posA.b])
            P.op("dve", lambda e: e.tensor_copy(out=fl(rk[0]), in_=fl(mskA)), reads=[mskA.b], writes=[rk[0].b])
            cur = 0
            for s_ in (1, 2, 4, 8, 16):
                a_, b_ = rk[cur], rk[1 - cur]
                P.op("dve", lambda e, a_=a_, b_=b_: e.tensor_copy(out=fl(b_), in_=fl(a_)), reads=[a_.b], writes=[b_.b])
                P.op("dve", lambda e, a_=a_, b_=b_, s_=s_: e.tensor_tensor(out=b_[:, :, s_:NE], in0=a_[:, :, s_:NE], in1=a_[:, :, 0:NE - s_], op=ALU.add),
                     reads=[a_.b], writes=[b_.b])
                cur = 1 - cur
            rnk = rk[cur]
            P.op("dve", lambda e: e.tensor_tensor(out=fl(rnk), in0=fl(rnk), in1=fl(mskA), op=ALU.subtract), reads=[rnk.b, mskA.b], writes=[rnk.b])
            for k in range(4):
                P.op("dve", lambda e, k=k: e.tensor_scalar(out=fl(selT), in0=fl(rnk), scalar1=float(k), scalar2=None, op0=ALU.is_equal), reads=[rnk.b], writes=[selT.b])
                P.op("dve", lambda e: e.tensor_tensor(out=fl(selT), in0=fl(selT), in1=fl(mskA), op=ALU.mult), reads=[selT.b, mskA.b], writes=[selT.b])
                P.op("dve", lambda e: e.tensor_tensor(out=fl(prodT), in0=fl(selT), in1=fl(posA), op=ALU.mult), reads=[selT.b, posA.b], writes=[prodT.b])
                P.op("dve", lambda e, k=k: e.reduce_sum(out=destF[:, k, :], in_=prodT[:], axis=AX.X), reads=[prodT.b], writes=[destF.b])
                P.op("dve", lambda e: e.tensor_tensor(out=fl(prodT), in0=fl(selT), in1=fl(wdense), op=ALU.mult), reads=[selT.b, wdense.b], writes=[prodT.b])
                P.op("dve", lambda e, k=k: e.reduce_sum(out=wkA[:, k, :], in_=prodT[:], axis=AX.X), reads=[prodT.b], writes=[wkA.b])
            P.op("dve", lambda e: e.tensor_copy(out=destI[:].rearrange("p a b -> p (a b)"), in_=destF[:].rearrange("p a b -> p (a b)")), reads=[destF.b], writes=[destI.b])
            P.op("dve", lambda e: e.memset(endS[:, 0:1], 0.0), writes=[endS.b])
            P.op("dve", lambda e: e.tensor_copy(out=endS[:, 1:NE], in_=endb[:, 0:NE - 1]), reads=[endb.b], writes=[endS.b])
            for j in range(2):
                P.op("dve", lambda e, j=j: e.tensor_scalar(out=ele[:], in0=endS[:], scalar1=bcol[:, j:j + 1], scalar2=None, op0=ALU.is_le),
                     reads=[endS.b, bcol.b], writes=[ele.b])
                for s_ in range(2):
                    P.op("dve", lambda e, s_=s_: e.tensor_tensor(out=elp[:], in0=ele[:], in1=parB[:, s_, :], op=ALU.mult), reads=[ele.b, parB.b], writes=[elp.b])
                    P.op("dve", lambda e, s_=s_: e.reduce_sum(out=nd[:, s_:s_ + 1], in_=elp[:], axis=AX.X), reads=[elp.b], writes=[nd.b])
                P.op("dve", lambda e, j=j: e.tensor_scalar(out=tblF[:, j, 0:2], in0=nd[:], scalar1=float(16 * 14), scalar2=None, op0=ALU.mult), reads=[nd.b], writes=[tblF.b])
                P.op("dve", lambda e, j=j: e.tensor_tensor(out=tblF[:, j, 2:3], in0=nd[:, 1:2], in1=nd[:, 0:1], op=ALU.subtract), reads=[nd.b], writes=[tblF.b])
                P.op("dve", lambda e, j=j: e.tensor_scalar(out=tblF[:, j, 2:3], in0=tblF[:, j, 2:3], scalar1=1.0, scalar2=None, op0=ALU.add), reads=[tblF.b], writes=[tblF.b])
                P.op("dve", lambda e, j=j: e.memset(tblF[:, j, 3:4], 0.0), writes=[tblF.b])
            P.op("dve", lambda e: e.tensor_copy(out=tbl[:].rearrange("p a b -> p (a b)"), in_=tblF[:].rearrange("p a b -> p (a b)")), reads=[tblF.b], writes=[tbl.b])
            P.op("dve", lambda e: e.tensor_scalar(out=qf[:], in0=endb[:], scalar1=5.0, scalar2=float(T0 + 4), op0=ALU.mult, op1=ALU.add), reads=[endb.b], writes=[qf.b])
            P.op("dve", lambda e: e.tensor_copy(out=endI[:], in_=qf[:]), reads=[qf.b], writes=[endI.b])
            if "tbl_d" in dbg:
                store("pool", tbl, dr["tbl_d"][:, 0:8], tbl[:].rearrange("p a b -> p (a b)"), [])
                store("pool", endI, dr["tbl_d"][:, 8:40], endI[:], [])
                store("pool", destI, dr["tbl_d"][:, 40:168], destI[:].rearrange("p a b -> p (a b)"), [])
            P.barrier()
            P.emit()
        if stop_after == 4:
            return nc

        NBLK = 160
        semw = [G.enter_context(nc.semaphore("semw%d" % i)) for i in range(2)]
        xs_gate = Buf("xs_gate")
        ysb_ = [Buf("ys%d" % b) for b in range(NBLK)]

        class Multi:
            def __init__(self, ins):
                self.ins = ins

            def then_inc(self, sem, n):
                for i_ in self.ins:
                    i_.then_inc(sem, n)

        with ExitStack() as SE:
            wslg = [sb(SE, "wslg%d" % i, [128, 9, 2 * D], BF16) for i in range(2)]
            wsld = [sb(SE, "wsld%d" % i, [128, 9, D], BF16) for i in range(2)]
            hrow = [sb(SE, "hrow%d" % i, [128, D], BF16) for i in range(4)]
            xrow = [sb(SE, "xrow%d" % i, [128, D], BF16) for i in range(2)]
            xT = [sb(SE, "xT%d" % i, [128, 8, 128], BF16) for i in range(2)]
            gt_ = sb(SE, "gt", [128, D], F32)
            sg_ = sb(SE, "sg", [128, D], F32)
            ut_ = sb(SE, "ut", [128, D], F32)
            act = [sb(SE, "act%d" % i, [128, D], BF16) for i in range(2)]
            actT = sb(SE, "actT", [128, 8, 128], BF16)
            ysb = [sb(SE, "ysb%d" % i, [128, D], F32) for i in range(2)]
            dmy = sb(SE, "dmy", [128, 8], F32)
            ptx = ps(SE, "ptx", [128, 8, 128], BF16)
            pta = ps(SE, "pta", [128, 8, 128], BF16)
            psGt = ps(SE, "psGt", [128, 2 * 512], F32)
            psUp = ps(SE, "psUp", [128, 2 * 512], F32)
            psY = ps(SE, "psY", [128, 2 * 512], F32)

            def wstream(g, lo=0, hi=NE):
                for ex_ in range(lo, hi):
                    s_ = ex_ % 2
                    if ex_ >= 2:
                        r = g.alloc_register("wr%d" % ex_)
                        g.reg_load(r, endI[0:1, ex_ - 2:ex_ - 1])
                        v = g.snap(r, donate=True)
                        g.wait_ge(P.sem["pe"], v)
                        g.free_register(r)
                    for k in range(8):
                        g.dma_start(out=wslg[s_][:, k, :], in_=dr["w_gu"][ex_, k * 128:(k + 1) * 128, :], max_dma_last_dim=8192).then_inc(semw[s_], 16)
                    g.dma_start(out=wslg[s_][0:1, 8, :], in_=dr["b_gu_r"][ex_:ex_ + 1, :], max_dma_last_dim=8192).then_inc(semw[s_], 16)
                    for kk in range(4):
                        g.dma_start(out=wsld[s_][:, 2 * kk:2 * kk + 2, :], in_=dr["w_dn"][ex_].rearrange("(k p) n -> p k n", p=128)[:, 2 * kk:2 * kk + 2, :]).then_inc(semw[s_], 16)
                    g.dma_start(out=wsld[s_][0:1, 8, :], in_=dr["b_dn"][ex_:ex_ + 1, :]).then_inc(semw[s_], 16)

            P.ops["pool"].append(lambda g: wstream(g, 0, 2))
            scs = [[Buf("scs%d_%d" % (a_, k_)) for k_ in range(4)] for a_ in range(4)]
            for i in range(NB):
                hr = hrow[i % 4]
                load("sp", hr, hr[:], dr["h2tok_d"][i * 128:(i + 1) * 128, :], [db["h2tok_d"][i]])
                for k in range(4):
                    P.dma("pool", lambda e, hr=hr, i=i, k=k: e.indirect_dma_start(
                        out=dr["xs_d"][:, :], out_offset=bass.IndirectOffsetOnAxis(ap=destI[:, k, i:i + 1], axis=0),
                        in_=hr[:], in_offset=None), scs[i % 4][k], reads=[hr.b, destI.b, xs_gate], writes=[])
            P.op("pool", lambda e: e.memset(dmy[:], 0.0), writes=[xs_gate])
            P.ops["pool"].append(lambda g: wstream(g, 2, NE))

            slotreg = {}

            def emit_tx(b):
                xr, xt_ = xrow[b % 2], xT[b % 2]
                load("sp", xr, xr[:], dr["xs_d"][b * 128:(b + 1) * 128, :], [xs_gate])

                def tr(e, xr=xr):
                    for k in range(8):
                        ins = e.transpose(out=ptx[:, k, :], in_=xr[:, k * 128:(k + 1) * 128], identity=identb[:])
                    return ins
                P.op("pe", tr, reads=[xr.b, identb.b], writes=[ptx.b])
                P.op("act", lambda e, xt_=xt_: e.copy(out=xt_[:], in_=ptx[:]), reads=[ptx.b], writes=[xt_.b])

            def emit_gu(b, parts=(0, 1)):
                xt_ = xT[b % 2]

                def gu_part(e, b=b, xt_=xt_, part=0):
                    p_, j_ = b % 128, b // 128
                    if part == 0:
                        regs = []
                        for c in range(3):
                            r = e.alloc_register("br%d_%d" % (b, c))
                            e.reg_load(r, tbl[p_:p_ + 1, j_, c:c + 1])
                            regs.append(r)
                        vals = [e.snap(r, donate=True) for r in regs]
                        e.wait_ge(semw[0], vals[0])
                        e.wait_ge(semw[1], vals[1])
                        e.free_register(regs[0])
                        e.free_register(regs[1])
                        slotreg[b] = (regs[2], vals[2])
                    sl = slotreg[b][1]
                    lasts = []

                    def body(s_):
                        pt_ = psGt if part == 0 else psUp
                        for n2 in range(2):
                            n = part * 2 + n2
                            o_ = pt_[:, n2 * 512:(n2 + 1) * 512]
                            for k in range(8):
                                e.matmul(o_, lhsT=xt_[:, k, :], rhs=wslg[s_][:, k, n * 512:(n + 1) * 512], start=(k == 0), stop=False)
                            ins = e.matmul(o_, lhsT=onesb[0:1, :], rhs=wslg[s_][0:1, 8, n * 512:(n + 1) * 512], start=False, stop=True)
                        return ins
                    with e.If(sl == 0):
                        lasts.append(body(0))
                    with e.Else():
                        lasts.append(body(1))
                    return Multi(lasts)
                if 0 in parts:
                    P.op("pe", lambda e: gu_part(e, part=0), reads=[xt_.b, onesb.b], writes=[psGt.b])
                if 1 in parts:
                    P.op("pe", lambda e: gu_part(e, part=1), reads=[xt_.b, onesb.b], writes=[psUp.b])

            def emit_ew_a(b, parts=(0, 1)):
                if 0 in parts:
                    P.op("dve", lambda e: e.tensor_scalar(out=gt_[:], in0=psGt[:], scalar1=7.0, scalar2=None, op0=ALU.min), reads=[psGt.b], writes=[gt_.b])
                    P.op("act", lambda e: e.activation(out=sg_[:], in_=gt_[:], func=AF.Sigmoid, scale=1.702), reads=[gt_.b], writes=[sg_.b])
                if 1 in parts:
                    P.op("dve", lambda e: e.tensor_scalar(out=ut_[:], in0=psUp[:], scalar1=7.0, scalar2=-7.0, op0=ALU.min, op1=ALU.max), reads=[psUp.b], writes=[ut_.b])

            def emit_ew_b(b):
                ab_ = act[b % 2]
                P.op("dve", lambda e: e.tensor_tensor(out=sg_[:], in0=gt_[:], in1=sg_[:], op=ALU.mult), reads=[gt_.b, sg_.b], writes=[sg_.b])
                P.op("dve", lambda e, ab_=ab_: e.scalar_tensor_tensor(out=ab_[:], in0=ut_[:], scalar=1.0, in1=sg_[:], op0=ALU.add, op1=ALU.mult),
                     reads=[ut_.b, sg_.b], writes=[ab_.b])

            def emit_ta(b):
                ab_ = act[b % 2]

                def tr(e, ab_=ab_):
                    for k in range(8):
                        ins = e.transpose(out=pta[:, k, :], in_=ab_[:, k * 128:(k + 1) * 128], identity=identb[:])
                    return ins
                P.op("pe", tr, reads=[ab_.b, identb.b], writes=[pta.b])
                P.op("act", lambda e: e.copy(out=actT[:], in_=pta[:]), reads=[pta.b], writes=[actT.b])

            def emit_down(b):
                yb = ysb[b % 2]

                def dn(e, b=b):
                    reg, sl = slotreg[b]
                    lasts = []

                    def body(s_):
                        for j in range(2):
                            o_ = psY[:, j * 512:(j + 1) * 512]
                            for k in range(8):
                                e.matmul(o_, lhsT=actT[:, k, :], rhs=wsld[s_][:, k, j * 512:(j + 1) * 512], start=(k == 0), stop=False)
                            ins = e.matmul(o_, lhsT=onesb[0:1, :], rhs=wsld[s_][0:1, 8, j * 512:(j + 1) * 512], start=False, stop=True)
                        return ins
                    with e.If(sl == 0):
                        lasts.append(body(0))
                    with e.Else():
                        lasts.append(body(1))
                    e.free_register(reg)
                    return Multi(lasts)
                P.op("pe", dn, reads=[actT.b, onesb.b], writes=[psY.b])
                P.op("act", lambda e, yb=yb: e.copy(out=yb[:], in_=psY[:]), reads=[psY.b], writes=[yb.b])
                P.dma("act", lambda e, yb=yb, b=b: e.dma_start(out=dr["ys_d"][b * 128:(b + 1) * 128, :], in_=yb[:]), yb.b, reads=[yb.b], writes=[ysb_[b]])

            assert P.tick["pe"] == T0
            dummy = lambda: P.op("pe", lambda e: e.transpose(out=ptx[:, 0, :], in_=identb[:], identity=identb[:]), reads=[identb.b], writes=[ptx.b])
            emit_tx(0)
            emit_tx(1)
            emit_gu(0)
            emit_ew_a(0)
            emit_ew_b(0)
            for b in range(NBLK):
                if b + 1 < NBLK:
                    emit_gu(b + 1, (0,))
                    emit_ew_a(b + 1, (0,))
                else:
                    dummy()
                emit_ta(b)
                if b + 1 < NBLK:
                    emit_gu(b + 1, (1,))
                    emit_ew_a(b + 1, (1,))
                else:
                    dummy()
                if b + 2 < NBLK:
                    emit_tx(b + 2)
                else:
                    dummy()
                emit_down(b)
                assert P.tick["pe"] == T0 + 4 + 5 * (b + 1)
                if b + 1 < NBLK:
                    emit_ew_b(b + 1)
            P.barrier()
            P.emit()
        if stop_after == 5:
            return nc

        with ExitStack() as SF:
            yk = [sb(SF, "yk%d" % i, [128, D], F32) for i in range(8)]
            accf = [sb(SF, "accf%d" % i, [128, D], F32) for i in range(2)]
            x1b = [sb(SF, "x1b%d" % i, [128, D], F32) for i in range(2)]
            tmpd = sb(SF, "tmpd", [128, D], F32)
            ob_ = [sb(SF, "obf%d" % i, [128, D], F32) for i in range(2)]
            junk = sb(SF, "junkf", [128, D], BF16)
            ss3 = sb(SF, "ss3", [128, 1], F32)
            def gathers(i):
                for k in range(4):
                    y_ = yk[(i * 4 + k) % 8]
                    P.dma("pool", lambda e, y_=y_, i=i, k=k: e.indirect_dma_start(
                        out=y_[:], out_offset=None, in_=dr["ys_d"][:, :],
                        in_offset=bass.IndirectOffsetOnAxis(ap=destI[:, k, i:i + 1], axis=0)), y_.b, reads=[destI.b], writes=[y_.b])
            gathers(0)
            tmps = [tmpd, sb(SF, "tmpe", [128, D], F32)]
            ss3s = [ss3, sb(SF, "ss3b", [128, 1], F32)]

            def fin_a(i):
                xb_, ac, tp, s3 = x1b[i % 2], accf[i % 2], tmps[i % 2], ss3s[i % 2]
                P.op("dve", lambda e: e.tensor_tensor(out=tp[:], in0=ac[:], in1=lateB[:, 0, :], op=ALU.mult), reads=[ac.b, lateB.b], writes=[tp.b])
                P.op("dve", lambda e: e.tensor_tensor(out=xb_[:], in0=tp[:], in1=xb_[:], op=ALU.add), reads=[tp.b, xb_.b], writes=[xb_.b])
                P.op("act", lambda e: e.activation(out=junk[:], in_=xb_[:], func=AF.Square, accum_out=s3[:]), reads=[xb_.b], writes=[junk.b, s3.b])

            def fin_b(i):
                xb_, o_, tp, s3 = x1b[i % 2], ob_[i % 2], tmps[i % 2], ss3s[i % 2]
                P.op("dve", lambda e: e.tensor_scalar(out=s3[:], in0=s3[:], scalar1=1.0 / D, scalar2=EPS, op0=ALU.mult, op1=ALU.add), reads=[s3.b], writes=[s3.b])
                P.op("act", lambda e: e.activation(out=s3[:], in_=s3[:], func=AF.Sqrt), reads=[s3.b], writes=[s3.b])
                P.op("dve", lambda e: e.reciprocal(out=s3[:], in_=s3[:]), reads=[s3.b], writes=[s3.b])
                P.op("dve", lambda e: e.scalar_tensor_tensor(out=tp[:], in0=xb_[:], scalar=s3[:, 0:1], in1=lateB[:, 1, :], op0=ALU.mult, op1=ALU.mult),
                     reads=[xb_.b, s3.b, lateB.b], writes=[tp.b])
                P.op("dve", lambda e: e.tensor_tensor(out=o_[:], in0=tp[:], in1=lateB[:, 2, :], op=ALU.add), reads=[tp.b, lateB.b], writes=[o_.b])
                store("sp", o_, dr["out"][i * 128:(i + 1) * 128, :], o_[:], [db["out"][i]])

            for i in range(NB):
                xb_, ac = x1b[i % 2], accf[i % 2]
                load("sp", xb_, xb_[:], dr["x1_d"][i], [db["x1_d"][i]])
                if i + 1 < NB:
                    gathers(i + 1)
                for k in range(4):
                    y_ = yk[(i * 4 + k) % 8]
                    if k == 0:
                        P.op("act", lambda e, y_=y_, ac=ac, i=i, k=k: e.activation(out=ac[:], in_=y_[:], func=AF.Identity, scale=wkA[:, k, i:i + 1]),
                             reads=[y_.b, wkA.b], writes=[ac.b])
                    else:
                        P.op("dve", lambda e, y_=y_, ac=ac, i=i, k=k: e.scalar_tensor_tensor(out=ac[:], in0=y_[:], scalar=wkA[:, k, i:i + 1], in1=ac[:], op0=ALU.mult, op1=ALU.add),
                             reads=[y_.b, wkA.b, ac.b], writes=[ac.b])
                fin_a(i)
                if i >= 1:
                    fin_b(i - 1)
            fin_b(NB - 1)
            P.barrier()
            P.emit()
    return nc


def _consts():
    rm = np.zeros((64, 64), np.float32)
    for m in range(32):
        rm[m + 32, m] = -1.0
        rm[m, m + 32] = 1.0
    invf = (1.0 / (10000.0 ** (np.arange(0, 64, 2, dtype=np.float32) / 64))).astype(np.float32)
    invf = np.tile(invf, 4).reshape(128, 1).astype(np.float32)
    t = np.arange(TW)
    rc0 = np.stack([1.0 / np.minimum(t[:16] + 1, w) for w in (2, 4, 8, 16)]).astype(np.float32).reshape(1, 64)
    return {
        "identb": np.eye(128, dtype=np.float32).astype(ml_dtypes.bfloat16),
        "identf": np.eye(128, dtype=np.float32),
        "rmat": rm.astype(ml_dtypes.bfloat16),
        "invf": invf, "rc0": rc0,
        "utri": np.triu(np.ones((128, 128), np.float32), 1).astype(ml_dtypes.bfloat16),
        "bcol": np.stack([np.arange(128), np.arange(128) + 128], 1).astype(np.float32),
        "par": np.concatenate([(np.arange(NE) % 2 == 0), (np.arange(NE) % 2 == 1)]).astype(np.float32).reshape(1, 2 * NE),
    }


def _colT(v, n):
    return np.ascontiguousarray(np.asarray(v, np.float32).reshape(n, 128).T)


def prep_shared(inp):
    f = lambda a: np.ascontiguousarray(np.asarray(a, np.float32))
    qperm = np.concatenate([np.arange(h * 192, h * 192 + 128) for h in range(8)] + [np.arange(h * 192 + 128, h * 192 + 192) for h in range(8)])
    kvperm = np.concatenate([np.arange(h * 256, h * 256 + 128) for h in range(8)] + [np.arange(h * 256 + 128, h * 256 + 256) for h in range(8)])
    guperm = np.concatenate([np.arange(0, 2 * D, 2), np.arange(1, 2 * D, 2)])
    b_gu = np.asarray(inp["b_gu"], np.float32)[0][:, guperm]
    b_gu_c = np.ascontiguousarray(b_gu.reshape(NE, 16, 128).transpose(2, 0, 1).reshape(128, NE * 16))
    sh = {
        "w_mod": f(inp["w_mod"][0]), "b_mod": f(inp["b_mod"][0]).reshape(1, -1), "g_mix": _colT(inp["g_mix"][0], 8),
        "w_in": f(inp["w_in"][0]), "b_gate": _colT(inp["b_gate"][0], 16),
        "w_grp": f(inp["w_pool_grp"][0]), "pscale": _colT(inp["pool_scale"][0], 8), "w_proj": f(inp["w_pool_out"][0]),
        "g_q": _colT(inp["g_q_a"][0], 6), "w_q": f(np.asarray(inp["w_q_b"][0])[:, qperm]),
        "g_kv": _colT(inp["g_kv_a"][0], 2), "w_kv": f(np.asarray(inp["w_kv_b"][0])[:, kvperm]),
        "w_mo": f(inp["w_mla_out"][0]), "w_out": f(inp["w_out"][0]), "g_ffn": f(inp["g_ffn"][0]).reshape(1, -1),
        "w_r": f(inp["w_router"][0]), "b_r": f(inp["b_router"][0]).reshape(1, -1),
        "w_gu": f(np.asarray(inp["w_gu"][0])[:, :, guperm]), "b_gu": b_gu_c, "b_gu_r": f(b_gu),
        "w_dn": f(inp["w_down"][0]), "b_dn": f(inp["b_down"][0]),
        "g_fin": f(inp["g_final"]).reshape(1, -1), "w_fmod": f(inp["w_fmod"]), "b_fmod": f(inp["b_fmod"]).reshape(1, -1),
    }
    sh.update(_consts())
    return sh


def core_inputs(inp, sh, b):
    m = dict(sh)
    m["x"] = np.ascontiguousarray(np.asarray(inp["x"][b], np.float32))
    m["cT"] = _colT(inp["c"][b], 8)
    m["pos"] = np.ascontiguousarray(np.asarray(inp["positions"][b], np.int32).reshape(1, S))
    return m


def kernel(**inputs):
    sh = prep_shared(inputs)
    nc = build()
    in_maps = [core_inputs(inputs, sh, b) for b in range(8)]
    res = run_bass_kernel_spmd(nc, in_maps, core_ids=list(range(8)))
    return np.stack([np.asarray(res.results[b]["out"], np.float32) for b in range(8)], axis=0)
```
